# Optimizing a Trainium2 kernel written in Bass

```python
import math
import jax, jax.numpy as jnp
from jax import lax
import numpy as np

D_MODEL = 1024
BATCH = 16
SEQ = 2048
DEPTH = 2

N_MIXERS = 2
N_ATTN_LAYERS = (DEPTH + 1) // 2
N_SSM_LAYERS = DEPTH // 2

N_HEADS = 16
N_KV_HEADS = 4
HEAD_DIM = 64
GQA_GROUP = N_HEADS // N_KV_HEADS
WINDOW = 128
ATTN_BLOCK = 128
QKV_DIM = (N_HEADS + 2 * N_KV_HEADS) * HEAD_DIM

REL_BUCKETS = 32
REL_MAX_DIST = 128

SSM_EXPAND = 2
D_INNER = SSM_EXPAND * D_MODEL
SSM_HEAD_DIM = 64
SSM_HEADS = D_INNER // SSM_HEAD_DIM
SSM_GROUPS = 4
HEADS_PER_GROUP = SSM_HEADS // SSM_GROUPS
D_STATE = 128
CONV_WIDTH = 4
CONV_DIM = D_INNER + 2 * SSM_GROUPS * D_STATE
SSM_IN_DIM = 2 * D_INNER + 2 * SSM_GROUPS * D_STATE + SSM_HEADS
SSM_CHUNK = 128

N_EXPERT_GROUPS = 8
EXPERTS_PER_GROUP = 8
N_EXPERTS = N_EXPERT_GROUPS * EXPERTS_PER_GROUP
TOP_K_IN_GROUP = 2
EXPERT_FF = D_MODEL // 2
MOE_BLOCK = 256

NORM_EPS = 1e-6

kernel_name = "hybrid_swa_ssd_hmoe"


def rmsnorm(x, w):
    xf = x.astype(jnp.float32)
    y = xf * lax.rsqrt(jnp.mean(xf * xf, axis=-1, keepdims=True) + NORM_EPS)
    return (y * w.astype(jnp.float32)).astype(x.dtype)


def t5_causal_bucket(dist):
    n = jnp.maximum(dist, 0)
    max_exact = REL_BUCKETS // 2
    nf = jnp.maximum(n, 1).astype(jnp.float32)
    large = max_exact + (jnp.log(nf / max_exact) / math.log(REL_MAX_DIST / max_exact)
                         * (REL_BUCKETS - max_exact)).astype(jnp.int32)
    large = jnp.minimum(large, REL_BUCKETS - 1)
    return jnp.where(n < max_exact, n, large)


def swa_sink_attention(h, w_qkv, sinks, w_o, rel_bias):
    b, L, _ = h.shape
    nb = L // ATTN_BLOCK
    qkv = h @ w_qkv
    q = qkv[..., :N_HEADS * HEAD_DIM].reshape(b, nb, ATTN_BLOCK, N_KV_HEADS, GQA_GROUP, HEAD_DIM)
    k = qkv[..., N_HEADS * HEAD_DIM:(N_HEADS + N_KV_HEADS) * HEAD_DIM]
    v = qkv[..., (N_HEADS + N_KV_HEADS) * HEAD_DIM:]
    k = k.reshape(b, nb, ATTN_BLOCK, N_KV_HEADS, HEAD_DIM)
    v = v.reshape(b, nb, ATTN_BLOCK, N_KV_HEADS, HEAD_DIM)
    pad = ((0, 0), (1, 0), (0, 0), (0, 0), (0, 0))
    k_band = jnp.concatenate([jnp.pad(k[:, :-1], pad), k], axis=2)
    v_band = jnp.concatenate([jnp.pad(v[:, :-1], pad), v], axis=2)

    scores = jnp.einsum('bnqhgd,bnkhd->bhgnqk', q, k_band,
                        preferred_element_type=jnp.float32) * (HEAD_DIM ** -0.5)

    qi = jnp.arange(ATTN_BLOCK)[:, None]
    ki = jnp.arange(2 * ATTN_BLOCK)[None, :]
    dist = qi + ATTN_BLOCK - ki
    in_window = (dist >= 0) & (dist < WINDOW)
    first_block = (jnp.arange(nb)[:, None] == 0) & (jnp.arange(2 * ATTN_BLOCK)[None, :] < ATTN_BLOCK)
    mask = in_window[None] & ~first_block[:, None, :]

    bias = rel_bias[t5_causal_bucket(dist)].astype(jnp.float32)
    bias = jnp.transpose(bias, (2, 0, 1)).reshape(N_KV_HEADS, GQA_GROUP, ATTN_BLOCK, 2 * ATTN_BLOCK)
    logits = jnp.where(mask, scores + bias[None, :, :, None], -jnp.inf)

    sink = sinks.astype(jnp.float32).reshape(N_KV_HEADS, GQA_GROUP)[None, :, :, None, None, None]
    m = jnp.maximum(jnp.max(logits, axis=-1, keepdims=True), sink)
    p = jnp.exp(logits - m)
    probs = p / (jnp.sum(p, axis=-1, keepdims=True) + jnp.exp(sink - m))

    out = jnp.einsum('bhgnqk,bnkhd->bnqhgd', probs.astype(v_band.dtype), v_band)
    return out.reshape(b, L, N_HEADS * HEAD_DIM) @ w_o


def causal_depthwise_conv(u, w, bias):
    y = lax.conv_general_dilated(u, w[:, None, :].astype(u.dtype), window_strides=(1,),
                                 padding=[(CONV_WIDTH - 1, 0)],
                                 dimension_numbers=('NWC', 'WIO', 'NWC'),
                                 feature_group_count=u.shape[-1])
    return y + bias.astype(u.dtype)


def ssd_chunked(X, A, Bm, Cm):
    b, L = X.shape[:2]
    nc = L // SSM_CHUNK
    X = X.astype(jnp.float32).reshape(b, nc, SSM_CHUNK, SSM_GROUPS, HEADS_PER_GROUP, SSM_HEAD_DIM)
    A = A.astype(jnp.float32).reshape(b, nc, SSM_CHUNK, SSM_GROUPS, HEADS_PER_GROUP)
    Bm = Bm.astype(jnp.float32).reshape(b, nc, SSM_CHUNK, SSM_GROUPS, D_STATE)
    Cm = Cm.astype(jnp.float32).reshape(b, nc, SSM_CHUNK, SSM_GROUPS, D_STATE)

    a_cs = jnp.cumsum(A, axis=2)
    seg = a_cs[:, :, :, None] - a_cs[:, :, None, :]
    causal = (jnp.arange(SSM_CHUNK)[:, None] >= jnp.arange(SSM_CHUNK)[None, :])[None, None, :, :, None, None]
    decay_ls = jnp.exp(jnp.where(causal, seg, -jnp.inf))

    cb = jnp.einsum('bclgn,bcsgn->bclsg', Cm, Bm)
    y_diag = jnp.einsum('bclsge,bcsgep->bclgep', cb[..., None] * decay_ls, X)

    decay_to_end = jnp.exp(a_cs[:, :, -1:] - a_cs)
    chunk_states = jnp.einsum('bclgn,bclgep->bcgepn', Bm, X * decay_to_end[..., None])
    chunk_decay = jnp.exp(a_cs[:, :, -1])

    def step(state, inp):
        dec, new = inp
        return dec[..., None, None] * state + new, state

    init = jnp.zeros((b, SSM_GROUPS, HEADS_PER_GROUP, SSM_HEAD_DIM, D_STATE), jnp.float32)
    _, states_in = lax.scan(step, init, (jnp.moveaxis(chunk_decay, 1, 0), jnp.moveaxis(chunk_states, 1, 0)))
    states_in = jnp.moveaxis(states_in, 0, 1)

    y_off = jnp.einsum('bclgn,bcgepn->bclgep', Cm, states_in) * jnp.exp(a_cs)[..., None]
    return (y_diag + y_off).reshape(b, L, SSM_HEADS, SSM_HEAD_DIM)


def mamba2_mixer(h, w_in, conv_w, conv_b, dt_bias, a_log, d_skip, norm_w, w_out):
    b, L, _ = h.shape
    zxbcdt = h @ w_in
    z = zxbcdt[..., :D_INNER]
    xbc = zxbcdt[..., D_INNER:D_INNER + CONV_DIM]
    dt = zxbcdt[..., D_INNER + CONV_DIM:]
    xbc = jax.nn.silu(causal_depthwise_conv(xbc, conv_w, conv_b))
    xs = xbc[..., :D_INNER].reshape(b, L, SSM_HEADS, SSM_HEAD_DIM)
    Bm = xbc[..., D_INNER:D_INNER + SSM_GROUPS * D_STATE].reshape(b, L, SSM_GROUPS, D_STATE)
    Cm = xbc[..., D_INNER + SSM_GROUPS * D_STATE:].reshape(b, L, SSM_GROUPS, D_STATE)

    dt = jax.nn.softplus(dt.astype(jnp.float32) + dt_bias.astype(jnp.float32))
    A = -jnp.exp(a_log.astype(jnp.float32))
    xf = xs.astype(jnp.float32)
    y = ssd_chunked(xf * dt[..., None], dt * A, Bm, Cm)
    y = y + xf * d_skip.astype(jnp.float32)[:, None]

    g = y.reshape(b, L, D_INNER) * jax.nn.silu(z.astype(jnp.float32))
    g = g.reshape(b, L, SSM_GROUPS, D_INNER // SSM_GROUPS)
    g = g * lax.rsqrt(jnp.mean(g * g, axis=-1, keepdims=True) + NORM_EPS)
    g = g.reshape(b, L, D_INNER) * norm_w.astype(jnp.float32)
    return g.astype(h.dtype) @ w_out


def hierarchical_moe(h, w_group, b_group, w_expert, b_expert, w_gate, w_up, w_down):
    b, L, dm = h.shape
    T = b * L
    xf = h.reshape(T, dm)

    g_prob = jax.nn.softmax((xf @ w_group).astype(jnp.float32) + b_group.astype(jnp.float32), axis=-1)
    g_p, g_idx = lax.top_k(g_prob, 1)
    e_logits = ((xf @ w_expert).astype(jnp.float32) + b_expert.astype(jnp.float32))
    e_logits = e_logits.reshape(T, N_EXPERT_GROUPS, EXPERTS_PER_GROUP)
    sel = jnp.take_along_axis(e_logits, g_idx[:, :, None], axis=1)[:, 0]
    e_p, e_idx = lax.top_k(jax.nn.softmax(sel, axis=-1), TOP_K_IN_GROUP)
    gates = e_p / jnp.sum(e_p, axis=-1, keepdims=True) * g_p
    expert_id = g_idx * EXPERTS_PER_GROUP + e_idx

    n_assign = T * TOP_K_IN_GROUP
    n_blocks = -(-n_assign // MOE_BLOCK) + N_EXPERTS
    n_slots = n_blocks * MOE_BLOCK
    flat_e = expert_id.reshape(-1).astype(jnp.int32)
    flat_tok = jnp.repeat(jnp.arange(T, dtype=jnp.int32), TOP_K_IN_GROUP)
    flat_w = gates.reshape(-1)
    order = jnp.argsort(flat_e)
    s_e = flat_e[order]
    counts = jnp.zeros((N_EXPERTS,), jnp.int32).at[flat_e].add(1)
    starts = jnp.cumsum(counts) - counts
    padded = (counts + MOE_BLOCK - 1) // MOE_BLOCK * MOE_BLOCK
    p_ends = jnp.cumsum(padded)
    p_starts = p_ends - padded
    dest = p_starts[s_e] + (jnp.arange(n_assign, dtype=jnp.int32) - starts[s_e])
    slot_tok = jnp.full((n_slots,), T, jnp.int32).at[dest].set(flat_tok[order])
    slot_w = jnp.zeros((n_slots,), jnp.float32).at[dest].set(flat_w[order])
    block_start = jnp.arange(n_blocks, dtype=jnp.int32) * MOE_BLOCK
    block_e = jnp.clip(jnp.searchsorted(p_ends, block_start, side='right'), 0, N_EXPERTS - 1).astype(jnp.int32)

    x_pad = jnp.concatenate([xf, jnp.zeros((1, dm), xf.dtype)], axis=0)

    def expert_block(args):
        tok, e = args
        xb = x_pad[tok]
        hid = jax.nn.silu(xb @ w_gate[e]) * (xb @ w_up[e])
        return hid @ w_down[e]

    y = lax.map(expert_block, (slot_tok.reshape(n_blocks, MOE_BLOCK), block_e)).reshape(n_slots, dm)
    out = jnp.zeros((T + 1, dm), y.dtype).at[slot_tok].add(y * slot_w[:, None].astype(y.dtype))
    return out[:T].reshape(b, L, dm)


def setup_inputs(seed: int = 0) -> dict:
    key = jax.random.key(seed)
    k = jax.random.split(key, 24)
    D = D_MODEL
    nrm = jax.random.normal
    dt = jnp.exp(jax.random.uniform(k[12], (N_SSM_LAYERS, SSM_HEADS)) * (math.log(0.1) - math.log(0.001)) + math.log(0.001))
    return {
        "x": nrm(k[0], (BATCH, SEQ, D), jnp.float32),
        "rel_bias": 0.5 * nrm(k[1], (REL_BUCKETS, N_HEADS), jnp.float32),
        "ln_mix": 1.0 + 0.05 * nrm(k[2], (DEPTH, D), jnp.float32),
        "ln_ffn": 1.0 + 0.05 * nrm(k[3], (DEPTH, D), jnp.float32),
        "ln_final": 1.0 + 0.05 * nrm(k[4], (D,), jnp.float32),
        "attn_w_qkv": nrm(k[5], (N_ATTN_LAYERS, D, QKV_DIM), jnp.float32) * D ** -0.5,
        "attn_sinks": 0.5 * nrm(k[6], (N_ATTN_LAYERS, N_HEADS), jnp.float32),
        "attn_w_o": nrm(k[7], (N_ATTN_LAYERS, N_HEADS * HEAD_DIM, D), jnp.float32) * (N_HEADS * HEAD_DIM) ** -0.5,
        "ssm_w_in": nrm(k[8], (N_SSM_LAYERS, D, SSM_IN_DIM), jnp.float32) * D ** -0.5,
        "ssm_conv_w": nrm(k[9], (N_SSM_LAYERS, CONV_WIDTH, CONV_DIM), jnp.float32) * CONV_WIDTH ** -0.5,
        "ssm_conv_b": 0.02 * nrm(k[10], (N_SSM_LAYERS, CONV_DIM), jnp.float32),
        "ssm_dt_bias": dt + jnp.log(-jnp.expm1(-dt)),
        "ssm_a_log": jnp.log(jax.random.uniform(k[13], (N_SSM_LAYERS, SSM_HEADS), jnp.float32, 1.0, 16.0)),
        "ssm_d": 1.0 + 0.1 * nrm(k[14], (N_SSM_LAYERS, SSM_HEADS), jnp.float32),
        "ssm_norm_w": 1.0 + 0.05 * nrm(k[15], (N_SSM_LAYERS, D_INNER), jnp.float32),
        "ssm_w_out": nrm(k[16], (N_SSM_LAYERS, D_INNER, D), jnp.float32) * D_INNER ** -0.5,
        "moe_w_group": nrm(k[17], (DEPTH, D, N_EXPERT_GROUPS), jnp.float32) * D ** -0.5,
        "moe_b_group": 0.01 * nrm(k[18], (DEPTH, N_EXPERT_GROUPS), jnp.float32),
        "moe_w_expert": nrm(k[19], (DEPTH, D, N_EXPERTS), jnp.float32) * D ** -0.5,
        "moe_b_expert": 0.01 * nrm(k[20], (DEPTH, N_EXPERTS), jnp.float32),
        "moe_w_gate": nrm(k[21], (DEPTH, N_EXPERTS, D, EXPERT_FF), jnp.float32) * D ** -0.5,
        "moe_w_up": nrm(k[22], (DEPTH, N_EXPERTS, D, EXPERT_FF), jnp.float32) * D ** -0.5,
        "moe_w_down": nrm(k[23], (DEPTH, N_EXPERTS, EXPERT_FF, D), jnp.float32) * EXPERT_FF ** -0.5,
    }


def reference(x, rel_bias, ln_mix, ln_ffn, ln_final, attn_w_qkv, attn_sinks, attn_w_o,
              ssm_w_in, ssm_conv_w, ssm_conv_b, ssm_dt_bias, ssm_a_log, ssm_d, ssm_norm_w, ssm_w_out,
              moe_w_group, moe_b_group, moe_w_expert, moe_b_expert, moe_w_gate, moe_w_up, moe_w_down):
    for i in range(DEPTH):
        h = rmsnorm(x, ln_mix[i])
        j = i // N_MIXERS
        if i % N_MIXERS == 0:
            x = x + swa_sink_attention(h, attn_w_qkv[j], attn_sinks[j], attn_w_o[j], rel_bias)
        else:
            x = x + mamba2_mixer(h, ssm_w_in[j], ssm_conv_w[j], ssm_conv_b[j], ssm_dt_bias[j],
                                 ssm_a_log[j], ssm_d[j], ssm_norm_w[j], ssm_w_out[j])
        h = rmsnorm(x, ln_ffn[i])
        x = x + hierarchical_moe(h, moe_w_group[i], moe_b_group[i], moe_w_expert[i], moe_b_expert[i],
                                 moe_w_gate[i], moe_w_up[i], moe_w_down[i])
    return rmsnorm(x, ln_final)
```

```python
import numpy as np
import concourse.bass as bass
import concourse.mybir as mybir
from concourse.bass_utils import run_bass_kernel_spmd

F32 = mybir.dt.float32
BF16 = mybir.dt.bfloat16
AF = mybir.ActivationFunctionType
ALU = mybir.AluOpType
AX = mybir.AxisListType

D = 1024
L = 2048
NT = 16
NE = 64
FF = 512
EPS = 1e-6
NEG = -30000.0


class Sched:
    def __init__(self, nc, self_sync=True):
        self.nc = nc
        self.eng = {"pe": nc.tensor, "act": nc.scalar, "dve": nc.vector, "pool": nc.gpsimd, "sp": nc.sync}
        self.sem = {}
        self.cnt = {}
        self.seen = {k: {} for k in self.eng}
        self.last_w = {}
        self.readers = {}
        self.self_sync = self_sync
        self._ctx = []
        self.n_ins = 0
        for k in self.eng:
            self._mk(k)

    def _mk(self, name):
        cm = self.nc.semaphore("s_" + name)
        s = cm.__enter__()
        self._ctx.append(cm)
        self.sem[name] = s
        self.cnt[name] = 0

    @staticmethod
    def _is_psum(k):
        return k in ("psT", "psT2") or (isinstance(k, tuple) and k[0] == "ps")

    def _deps(self, reads, writes, en=None):
        deps = {}
        for k in reads:
            t = self.last_w.get(k)
            if t is not None:
                deps[t[0]] = max(deps.get(t[0], 0), t[1])
            if self._is_psum(k):
                for s, i in self.readers.get(k, {}).items():
                    if s != en:
                        deps[s] = max(deps.get(s, 0), i)
        for k in writes:
            t = self.last_w.get(k)
            if t is not None:
                deps[t[0]] = max(deps.get(t[0], 0), t[1])
            for s, i in self.readers.get(k, {}).items():
                deps[s] = max(deps.get(s, 0), i)
        return deps

    def _wait(self, en, deps):
        e = self.eng[en]
        for src, idx in deps.items():
            if src == en and (en == "pe" or not self.self_sync):
                continue
            if self.seen[en].get(src, 0) >= idx:
                continue
            e.wait_ge(self.sem[src], idx)
            self.seen[en][src] = idx

    def _record(self, tag, reads, writes):
        for k in reads:
            r = self.readers.setdefault(k, {})
            r[tag[0]] = max(r.get(tag[0], 0), tag[1])
        for k in writes:
            self.last_w[k] = tag
            self.readers[k] = {}

    def record(self, f):
        self.rec = []
        f()
        lst, self.rec = self.rec, None
        return lst

    def emit_interleaved(self, lists):
        its = [list(l) for l in lists if l]
        pos = [0] * len(its)
        while any(p < len(l) for p, l in zip(pos, its)):
            for i, l in enumerate(its):
                while pos[i] < len(l):
                    o = l[pos[i]]
                    pos[i] += 1
                    if o[0] == "op":
                        self.op(*o[1:])
                        if not (o[1] == "pe" and o[5] is False):
                            break
                    else:
                        self.dma(*o[1:])
                        break

    def op(self, en, fn, reads=(), writes=(), inc=True):
        if getattr(self, "rec", None) is not None:
            self.rec.append(("op", en, fn, tuple(reads), tuple(writes), inc))
            return None
        self._wait(en, self._deps(reads, writes, en))
        ins = fn(self.eng[en])
        self.n_ins += 1
        if inc:
            ins.then_inc(self.sem[en], 1)
            self.cnt[en] += 1
            tag = (en, self.cnt[en])
        else:
            tag = (en, self.cnt[en] + 1)
        self._record(tag, reads, writes)
        return ins

    def dma(self, qn, dsem, out, in_, reads=(), writes=()):
        if getattr(self, "rec", None) is not None:
            self.rec.append(("dma", qn, dsem, out, in_, tuple(reads), tuple(writes)))
            return None
        if dsem not in self.sem:
            self._mk(dsem)
        deps = self._deps(reads, writes)
        if self.cnt[dsem] > 0:
            deps[dsem] = max(deps.get(dsem, 0), self.cnt[dsem])
        self._wait(qn, deps)
        ins = self.eng[qn].dma_start(out=out, in_=in_)
        ins.then_inc(self.sem[dsem], 16)
        self.n_ins += 1
        self.cnt[dsem] += 16
        self._record((dsem, self.cnt[dsem]), reads, writes)
        return ins

    def barrier(self):
        for en in self.eng:
            deps = {src: c for src, c in self.cnt.items() if c > 0}
            self._wait(en, deps)

    def wait_keys(self, en, keys):
        deps = {}
        for k in keys:
            t = self.last_w.get(k)
            if t is not None:
                deps[t[0]] = max(deps.get(t[0], 0), t[1])
            for s, i in self.readers.get(k, {}).items():
                deps[s] = max(deps.get(s, 0), i)
        self._wait(en, deps)


class K:
    def __init__(self, n_seq=2, phases=("attn", "moe0", "ssm", "moe1", "final"), n_exp=NE, debug_x=False):
        self.n_seq = n_seq
        self.phases = phases
        self.n_exp = n_exp
        nc = self.nc = bass.Bass("TRN2", target_bir_lowering=False)
        self.S = Sched(nc)
        self._cms = []
        dt = nc.dram_tensor
        self.x_in = dt("x", [n_seq, L, D], F32, kind="ExternalInput").ap()
        self.out = dt("out", [n_seq, L, D], F32, kind="ExternalOutput").ap()
        self.ln_mix = dt("ln_mix", [2, D], F32, kind="ExternalInput").ap()
        self.ln_ffn = dt("ln_ffn", [2, D], F32, kind="ExternalInput").ap()
        self.ln_final = dt("ln_final", [1, D], F32, kind="ExternalInput").ap()
        self.ident_d = dt("ident", [128, 128], F32, kind="ExternalInput").ap()
        if "attn" in phases:
          self.w_qkv = dt("attn_w_qkv", [D, 1536], F32, kind="ExternalInput").ap()
          self.w_o = dt("attn_w_o", [D, D], F32, kind="ExternalInput").ap()
          self.sinks = dt("attn_sinks", [1, 16], F32, kind="ExternalInput").ap()
          self.tcur_d = dt("tcur", [128, 16, 128], F32, kind="ExternalInput").ap()
          self.tprev_d = dt("tprev", [128, 16, 128], F32, kind="ExternalInput").ap()
        if "ssm" in phases:
          self.w_in = dt("ssm_w_in", [D, 5152], F32, kind="ExternalInput").ap()
          self.w_out = dt("ssm_w_out", [2048, D], F32, kind="ExternalInput").ap()
          self.cwT_d = dt("ssm_cwT", [128, 24, 4], F32, kind="ExternalInput").ap()
          self.cb_d = dt("ssm_cb", [128, 24], F32, kind="ExternalInput").ap()
          self.hcol_d = dt("ssm_hcol", [32, 2], F32, kind="ExternalInput").ap()
          self.dsk_d = dt("ssm_d", [1, 32], F32, kind="ExternalInput").ap()
          self.nw_d = dt("ssm_norm_w", [1, 2048], F32, kind="ExternalInput").ap()
          self.uinc_d = dt("c_uincl", [128, 128], F32, kind="ExternalInput").ap()
          self.ones_d = dt("c_ones", [128, 128], F32, kind="ExternalInput").ap()
          self.negm_d = dt("c_negmask4", [128, 512], F32, kind="ExternalInput").ap()
        self.w_router = dt("w_router", [2, D, 72], F32, kind="ExternalInput").ap()
        self.b_router = dt("b_router", [2, 72], F32, kind="ExternalInput").ap()
        self.w_gate = dt("moe_w_gate", [2, n_exp, D, FF], F32, kind="ExternalInput").ap()
        self.w_up = dt("moe_w_up", [2, n_exp, D, FF], F32, kind="ExternalInput").ap()
        self.w_down = dt("moe_w_down", [2, n_exp, FF, D], F32, kind="ExternalInput").ap()

    def sb(self, name, shape, dtype=F32):
        self._uid = getattr(self, "_uid", 0) + 1
        cm = self.nc.sbuf_tensor(f"{name}_{self._uid}", shape, dtype)
        t = cm.__enter__()
        self._cms.append(cm)
        return t

    def pst(self, name, shape, dtype=F32):
        cm = self.nc.psum_tensor(name, shape, dtype)
        t = cm.__enter__()
        self._cms.append(cm)
        return t

    def scope_begin(self):
        return len(self._cms)

    def scope_end(self, mark):
        self.S.barrier()
        while len(self._cms) > mark:
            self._cms.pop().__exit__(None, None, None)

    def load_lnw(self, src_row):
        self.lnw = self.sb("lnw", [128, D], F32)
        self.hn = [self.sb(f"hn{i}", [128, D], F32) for i in range(2)]
        self.S.dma("sp", "d_const", self.lnw[:], src_row.partition_broadcast(128), writes=["lnw"])

    def norm_all(self, per_tile):
        for t0 in range(0, NT, 2):
            recs = [self.S.record(lambda tt=tt: per_tile(tt)) for tt in (t0, t0 + 1)]
            self.S.emit_interleaved(recs)

    def mm_group(self, out, pairs, reads, writes):
        n = len(pairs)
        for i, (lhsT, rhs) in enumerate(pairs):
            self.S.op("pe", lambda e, lhsT=lhsT, rhs=rhs, i=i: e.matmul(out, lhsT, rhs, start=(i == 0), stop=(i == n - 1)),
                      reads=reads, writes=writes, inc=(i == n - 1))

    def build(self):
        S = self.S
        nc = self.nc
        self.x = self.sb("xres", [128, NT, D], F32)
        self.hT = self.sb("hT", [128, 8, L], BF16)
        self.ident = self.sb("ident_sb", [128, 128], F32)
        self.sm = [self.sb(f"sm{i}", [128, 512], F32) for i in range(2)]
        self.epsb = self.sb("epsb", [128, 1], F32)
        S.op("dve", lambda e: e.memset(self.epsb[:], EPS), writes=["epsb"])
        self.oneb = self.sb("oneb", [128, 1], F32)
        S.op("dve", lambda e: e.memset(self.oneb[:], 1.0), writes=["oneb"])
        self.psT = self.pst("psT", [128, 1024], F32)
        self.ps = [self.pst(f"ps{i}", [128, 512], F32) for i in range(6)]

        S.dma("sp", "d_const", self.ident[:], self.ident_d, writes=["ident"])

        for s in range(self.n_seq):
            xin = self.x_in[s].rearrange("(tt p) d -> p tt d", p=128)
            for tt in range(NT):
                S.dma("sp", f"d_x{tt % 8}", self.x[:, tt, :], xin[:, tt, :], writes=[("x", tt)])
            for ph in self.phases:
                if ph == "attn":
                    self.attn_phase()
                elif ph == "ssm":
                    self.ssm_phase()
                elif ph == "moe0":
                    self.moe_phase(0)
                elif ph == "moe1":
                    self.moe_phase(1)
                elif ph == "final":
                    self.final_phase(s)
            if "final" not in self.phases:
                xo = self.out[s].rearrange("(tt p) d -> p tt d", p=128)
                for tt in range(NT):
                    S.dma("sp", f"d_o{tt % 4}", xo[:, tt, :], self.x[:, tt, :], reads=[("x", tt)])
        S.wait_keys("sp", [("x", tt) for tt in range(NT)] + ["fin0", "fin1"])
        return nc

    def norm_tile(self, tt, want32=None):
        S = self.S
        p = tt % 2
        sm = self.sm[p]
        hn = self.hn[p]
        xk = ("x", tt)
        S.op("act", lambda e: e.activation(out=hn[:], in_=self.x[:, tt, :], func=AF.Square, accum_out=sm[:, 0:1]),
             reads=[xk], writes=[("sm", p), ("hn", p)])
        S.op("act", lambda e: e.activation(out=sm[:, 1:2], in_=sm[:, 0:1], func=AF.Sqrt, scale=1.0 / D, bias=self.epsb[:, 0:1]),
             reads=[("sm", p), "epsb"], writes=[("sm", p)])
        S.op("dve", lambda e: e.reciprocal(out=sm[:, 2:3], in_=sm[:, 1:2]), reads=[("sm", p)], writes=[("sm", p)])
        S.op("dve", lambda e: e.scalar_tensor_tensor(out=hn[:], in0=self.x[:, tt, :], scalar=sm[:, 2:3], in1=self.lnw[:],
                                                     op0=ALU.mult, op1=ALU.mult),
             reads=[xk, ("sm", p), "lnw"], writes=[("hn", p)])
        if p == 0:
            banks = [(self.psT[:, 0:512], "psT"), (self.psT[:, 512:1024], "psT2")]
        else:
            banks = [(self.ps[0][:], ("ps", 0)), (self.ps[1][:], ("ps", 1))]
        for h, (bk, bkey) in enumerate(banks):
            for i in range(4):
                kc = 4 * h + i
                S.op("pe", lambda e, kc=kc, i=i, bk=bk: e.transpose(bk[:, i * 128:(i + 1) * 128], hn[:, kc * 128:(kc + 1) * 128], self.ident[:]),
                     reads=[("hn", p), "ident"], writes=[bkey], inc=(i == 3))
            S.op("act", lambda e, h=h, bk=bk: e.activation(out=self.hT[:, 4 * h:4 * h + 4, tt * 128:(tt + 1) * 128],
                                                           in_=bk.rearrange("p (k t) -> p k t", k=4), func=AF.Copy),
                 reads=[bkey], writes=[("hT", tt)])
            if want32 is not None:
                S.op("act", lambda e, h=h, bk=bk: e.activation(out=want32[0][:, 4 * h:4 * h + 4, :], in_=bk.rearrange("p (k t) -> p k t", k=4), func=AF.Copy),
                     reads=[bkey], writes=[want32[1]])

    def load_w(self, dst, src2d, key, KC, F):
        S = self.S
        g = max(1, 2048 // F)
        for k0 in range(0, KC, g):
            kn = min(g, KC - k0)
            si = self.stg_i % 2
            self.stg_i += 1
            view = self.stg[si][:, 0:kn * F].rearrange("p (a b) -> p a b", a=kn)
            src = src2d[k0 * 128:(k0 + kn) * 128, :].rearrange("(kc p) f -> p kc f", p=128)
            S.dma("sp", f"d_stg{si}", view, src, writes=[("stg", si)])
            d = dst[:, k0:k0 + kn, :]
            S.op("pool", lambda en, d=d, view=view: en.tensor_copy(out=d, in_=view), reads=[("stg", si)], writes=[key])

    def attn_phase(self):
        S = self.S
        m0 = self.scope_begin()
        self.load_lnw(self.ln_mix[0:1, :])
        self.norm_all(self.norm_tile)
        self.scope_end(m0)
        KT = self.sb("KT", [128, 4, L], BF16)
        Va = self.sb("Vaug", [128, NT, 4, 65], BF16)
        Wq = self.sb("Wq", [128, 8, 1024], BF16)
        Wo = self.sb("Wo", [128, 8, 1024], BF16)
        identb = self.sb("identb", [128, 128], BF16)
        S.op("dve", lambda e: e.tensor_copy(out=identb[:], in_=self.ident[:]), reads=["ident"], writes=["identb"])
        S.op("dve", lambda e: e.memset(Va[:], 1.0), writes=["Va"])
        m1 = self.scope_begin()
        self.stg = [self.sb(f"stg{i}", [128, 2048], F32) for i in range(2)]
        self.stg_i = 0
        Wkv = self.sb("Wkv", [128, 8, 512], BF16)
        self.load_w(Wkv, self.w_qkv[:, 1024:1536], "Wkv", 8, 512)
        self.load_w(Wq, self.w_qkv[:, 0:1024], "Wq", 8, 1024)
        self.load_w(Wo, self.w_o, "Wo", 8, 1024)
        for ct in range(4):
            hk = [("hT", 4 * ct + i) for i in range(4)]
            for j in range(4):
                pk = self.ps[j % 2]
                self.mm_group(pk[0:64, :], [(Wkv[:, kc, j * 64:(j + 1) * 64], self.hT[:, kc, ct * 512:(ct + 1) * 512]) for kc in range(8)],
                              reads=hk + ["Wkv"], writes=[("ps", j % 2)])
                S.op("act", lambda e, pk=pk, j=j, ct=ct: e.activation(out=KT[0:64, j, ct * 512:(ct + 1) * 512], in_=pk[0:64, :], func=AF.Copy),
                     reads=[("ps", j % 2)], writes=["KT"])
        for tt in range(NT):
            pv = self.ps[2 + tt % 2]
            self.mm_group(pv[:, 0:256], [(self.hT[:, kc, tt * 128:(tt + 1) * 128], Wkv[:, kc, 256:512]) for kc in range(8)],
                          reads=[("hT", tt), "Wkv"], writes=[("ps", 2 + tt % 2)])
            S.op("act", lambda e, pv=pv, tt=tt: e.activation(out=Va[:, tt, :, 0:64], in_=pv[:, 0:256].rearrange("p (j d) -> p j d", j=4), func=AF.Copy),
                 reads=[("ps", 2 + tt % 2)], writes=["Va"])
        self.scope_end(m1)
        Tc = self.sb("Tc", [128, 16, 128], F32)
        Tp = self.sb("Tp", [128, 16, 128], F32)
        S.dma("sp", "d_const", Tc[:], self.tcur_d, writes=["Tc"])
        S.dma("sp", "d_const", Tp[:], self.tprev_d, writes=["Tp"])
        esk = self.sb("esk", [128, 16], F32)
        S.dma("sp", "d_const", esk[:], self.sinks[0:1, :].partition_broadcast(128), writes=["esk"])
        S.op("act", lambda e: e.activation(out=esk[:], in_=esk[:], func=AF.Exp), reads=["esk"], writes=["esk"])
        QT = [self.sb(f"QT{i}", [128, 16, 128], BF16) for i in range(2)]
        tA = [self.sb(f"tA{i}", [128, 512], F32) for i in range(2)]
        PA = [self.sb(f"PA{i}", [128, 512], BF16) for i in range(2)]
        PB = [self.sb(f"PB{i}", [128, 512], BF16) for i in range(2)]
        at = [self.sb(f"attn_tok{i}", [128, 16, 64], BF16) for i in range(2)]
        aT = self.sb("attnT", [128, 8, 128], BF16)
        den = self.sb("den", [128, 8], F32)
        ps = self.ps

        def st_q(b):
            qt, qk = QT[b % 2], ("QT", b % 2)
            pq = ps[4]
            for j in range(4):
                for g in range(4):
                    h = 4 * j + g
                    self.mm_group(pq[0:64, g * 128:(g + 1) * 128],
                                  [(Wq[:, kc, h * 64:(h + 1) * 64], self.hT[:, kc, b * 128:(b + 1) * 128]) for kc in range(8)],
                                  reads=[("hT", b), "Wq"], writes=[("ps", 4)])
                S.op("act", lambda e, j=j: e.activation(out=qt[0:64, 4 * j:4 * j + 4, :], in_=pq[0:64, :].rearrange("p (g t) -> p g t", g=4),
                                                        func=AF.Copy, scale=0.125),
                     reads=[("ps", 4)], writes=[qk])

        def st_s(b):
            qt, qk = QT[b % 2], ("QT", b % 2)
            ab, abk = at[b % 2], ("at", b % 2)
            for j in range(4):
                po = ps[2 + j % 2]
                pok = ("ps", 2 + j % 2)
                srcs = [(b, Tc, PA[j % 2], ("PA", j % 2), 0)]
                if b > 0:
                    srcs.append((b - 1, Tp, PB[j % 2], ("PB", j % 2), 1))
                for kb, T, P, pkey, w in srcs:
                    pa = ps[w]
                    S.op("pe", lambda e, pa=pa, kb=kb, j=j: e.matmul(pa[:], KT[0:64, j, kb * 128:(kb + 1) * 128], qt[0:64, 4 * j:4 * j + 4, :], start=True, stop=True),
                         reads=["KT", qk], writes=[("ps", w)])
                    S.op("dve", lambda e, pa=pa, T=T, w=w, j=j: e.tensor_tensor(out=tA[w][:], in0=pa[:], in1=T[:, 4 * j:4 * j + 4, :].rearrange("p g t -> p (g t)"), op=ALU.add),
                         reads=[("ps", w), "Tc", "Tp"], writes=[("tA", w)])
                    S.op("act", lambda e, P=P, w=w: e.activation(out=P[:], in_=tA[w][:], func=AF.Exp), reads=[("tA", w)], writes=[pkey])
                for g in range(4):
                    o = po[:, g * 65:(g + 1) * 65]
                    S.op("pe", lambda e, o=o, g=g, j=j: e.matmul(o, PA[j % 2][:, g * 128:(g + 1) * 128], Va[:, b, j, :], start=True, stop=(b == 0)),
                         reads=[("PA", j % 2), "Va"], writes=[pok], inc=(b == 0 and g == 3))
                    if b > 0:
                        S.op("pe", lambda e, o=o, g=g, j=j: e.matmul(o, PB[j % 2][:, g * 128:(g + 1) * 128], Va[:, b - 1, j, :], start=False, stop=True),
                             reads=[("PB", j % 2), "Va"], writes=[pok], inc=(g == 3))
                pov = po[:, 0:260].rearrange("p (g c) -> p g c", g=4)
                dj = den[:, 0:4]
                S.op("dve", lambda e, pov=pov, j=j: e.tensor_tensor(out=dj, in0=pov[:, :, 64], in1=esk[:, 4 * j:4 * j + 4], op=ALU.add),
                     reads=[pok, "esk"], writes=["den"])
                S.op("dve", lambda e: e.reciprocal(out=den[:, 4:8], in_=dj), reads=["den"], writes=["den"])
                S.op("dve", lambda e, pov=pov, j=j: e.tensor_tensor(out=ab[:, 4 * j:4 * j + 4, :], in0=pov[:, :, 0:64],
                                                                   in1=den[:, 4:8].unsqueeze(2).broadcast_to([128, 4, 64]), op=ALU.mult),
                     reads=[pok, "den"], writes=[abk])

        def st_o(b):
            ab, abk = at[b % 2], ("at", b % 2)
            for kc in range(8):
                S.op("pe", lambda e, kc=kc: e.matmul(self.psT[:, kc * 128:(kc + 1) * 128], ab[:].rearrange("p h d -> p (h d)")[:, kc * 128:(kc + 1) * 128],
                                                    identb[:], start=True, stop=True),
                     reads=[abk, "identb"], writes=["psT"], inc=(kc == 7))
            S.op("act", lambda e: e.activation(out=aT[:], in_=self.psT[:].rearrange("p (k t) -> p k t", k=8), func=AF.Copy), reads=["psT"], writes=["aT"])
            for half in range(2):
                pw = ps[5]
                self.mm_group(pw[:], [(aT[:, kc, :], Wo[:, kc, half * 512:(half + 1) * 512]) for kc in range(8)], reads=["aT", "Wo"], writes=[("ps", 5)])
                xs = self.x[:, b, half * 512:(half + 1) * 512]
                S.op("dve", lambda e, xs=xs: e.tensor_tensor(out=xs, in0=pw[:], in1=xs, op=ALU.add), reads=[("ps", 5), ("x", b)], writes=[("x", b)])

        for step in range(NT + 2):
            lists = []
            if 0 <= step - 2 < NT:
                lists.append(S.record(lambda bb=step - 2: st_o(bb)))
            if 0 <= step - 1 < NT:
                lists.append(S.record(lambda bb=step - 1: st_s(bb)))
            if step < NT:
                lists.append(S.record(lambda bb=step: st_q(bb)))
            S.emit_interleaved(lists)
        self.scope_end(m0)

    def ssm_phase(self):
        S = self.S
        m0 = self.scope_begin()
        self.load_lnw(self.ln_mix[1:2, :])
        self.norm_all(self.norm_tile)
        self.scope_end(m0)
        ps = self.ps
        sbf = self.sb
        identb = sbf("identb", [128, 128], BF16)
        S.op("dve", lambda e: e.tensor_copy(out=identb[:], in_=self.ident[:]), reads=["ident"], writes=["identb"])
        uinc = sbf("uinc", [128, 128], F32)
        ones = sbf("ones", [128, 128], F32)
        negm = sbf("negm", [128, 512], F32)
        cwT = sbf("cwT", [128, 24, 4], F32)
        cb = sbf("cb", [128, 24], F32)
        hcol = sbf("hcol", [32, 4], F32)
        dbc = sbf("dbc", [128, 32], F32)
        for t, src, k in ((uinc, self.uinc_d, "uinc"), (ones, self.ones_d, "ones"), (negm, self.negm_d, "negm"), (cwT, self.cwT_d, "cwT"),
                          (cb, self.cb_d, "cb"), (dbc, self.dsk_d[0:1, :].partition_broadcast(128), "dbc")):
            S.dma("sp", "d_const", t[:], src, writes=[k])
        S.dma("sp", "d_const", hcol[:, 0:2], self.hcol_d, writes=["hcol"])
        S.op("act", lambda e: e.activation(out=hcol[:, 2:3], in_=hcol[:, 1:2], func=AF.Exp), reads=["hcol"], writes=["hcol"])
        S.op("dve", lambda e: e.tensor_scalar(out=hcol[:, 3:4], in0=hcol[:, 2:3], scalar1=-1.0, scalar2=None, op0=ALU.mult), reads=["hcol"], writes=["hcol"])
        dt_tok = sbf("dt_tok", [128, NT, 32], F32)
        nacs = sbf("nacs", [128, NT, 32], F32)
        ea = sbf("ea", [128, NT, 32], F32)
        dec = sbf("dec", [128, NT, 32], F32)
        dtdte = sbf("dtdte", [128, NT, 32], F32)
        m1 = self.scope_begin()
        self.stg = [sbf(f"stg{i}", [128, 2048], F32) for i in range(2)]
        self.stg_i = 0
        Wdt = sbf("Wdt", [128, 8, 32], BF16)
        self.load_w(Wdt, self.w_in[:, 5120:5152], "Wdt", 8, 32)
        dtT = sbf("dtT", [128, L], F32)
        dAT = sbf("dAT", [128, L], F32)
        dA_tok = sbf("dA_tok", [128, NT, 32], F32)
        tmp = sbf("tmp_s", [128, NT, 32], F32)
        for ct in range(4):
            p = ps[ct % 2]
            sl = slice(ct * 512, (ct + 1) * 512)
            self.mm_group(p[0:32, :], [(Wdt[:, kc, :], self.hT[:, kc, sl]) for kc in range(8)],
                          reads=[("hT", 4 * ct + i) for i in range(4)] + ["Wdt"], writes=[("ps", ct % 2)])
            S.op("act", lambda e, p=p, sl=sl: e.activation(out=dtT[0:32, sl], in_=p[0:32, :], func=AF.Exp, bias=hcol[:, 0:1], scale=1.0),
                 reads=[("ps", ct % 2), "hcol"], writes=["dtT"])
            S.op("act", lambda e, sl=sl: e.activation(out=dtT[0:32, sl], in_=dtT[0:32, sl], func=AF.Ln, bias=self.oneb[0:32, 0:1], scale=1.0),
                 reads=["dtT", "oneb"], writes=["dtT"])
            S.op("dve", lambda e, sl=sl: e.tensor_scalar(out=dAT[0:32, sl], in0=dtT[0:32, sl], scalar1=hcol[:, 3:4], scalar2=None, op0=ALU.mult),
                 reads=["dtT", "hcol"], writes=["dAT"])
        for srcT, dst, k, bank in ((dtT, dt_tok, "dt_tok", 2), (dAT, dA_tok, "dA_tok", 3)):
            for c in range(NT):
                S.op("pe", lambda e, srcT=srcT, c=c, bank=bank: e.matmul(ps[bank][:, c * 32:(c + 1) * 32], srcT[0:32, c * 128:(c + 1) * 128],
                                                                        self.ident[0:32, 0:32], start=True, stop=True),
                     reads=["dtT", "dAT", "ident"], writes=[("ps", bank)], inc=(c == NT - 1))
            S.op("act", lambda e, dst=dst, bank=bank: e.activation(out=dst[:].rearrange("p c h -> p (c h)"), in_=ps[bank][:], func=AF.Copy),
                 reads=[("ps", bank)], writes=[k])
        for c in range(NT):
            S.op("pe", lambda e, c=c: e.matmul(ps[4][:, c * 32:(c + 1) * 32], uinc[:], dA_tok[:, c, :], start=True, stop=True),
                 reads=["uinc", "dA_tok"], writes=[("ps", 4)], inc=(c == NT - 1))
        for c in range(NT):
            S.op("pe", lambda e, c=c: e.matmul(ps[5][:, c * 32:(c + 1) * 32], ones[:], dA_tok[:, c, :], start=True, stop=True),
                 reads=["ones", "dA_tok"], writes=[("ps", 5)], inc=(c == NT - 1))
        fl = lambda t: t[:].rearrange("p c h -> p (c h)")
        S.op("act", lambda e: e.activation(out=fl(ea), in_=ps[4][:], func=AF.Exp), reads=[("ps", 4)], writes=["ea"])
        S.op("dve", lambda e: e.tensor_scalar(out=fl(nacs), in0=ps[4][:], scalar1=-1.0, scalar2=None, op0=ALU.mult), reads=[("ps", 4)], writes=["nacs"])
        S.op("act", lambda e: e.activation(out=fl(dec), in_=ps[5][:], func=AF.Exp), reads=[("ps", 5)], writes=["dec"])
        S.op("dve", lambda e: e.tensor_tensor(out=fl(tmp), in0=ps[5][:], in1=fl(nacs), op=ALU.add), reads=[("ps", 5), "nacs"], writes=["tmp_s"])
        S.op("act", lambda e: e.activation(out=fl(tmp), in_=fl(tmp), func=AF.Exp), reads=["tmp_s"], writes=["tmp_s"])
        S.op("dve", lambda e: e.tensor_tensor(out=fl(dtdte), in0=fl(tmp), in1=fl(dt_tok), op=ALU.mult), reads=["tmp_s", "dt_tok"], writes=["dtdte"])
        self.scope_end(m1)
        dA_tok2 = sbf("dA_tok2", [128, NT, 32], F32)
        abc = sbf("abc", [128, 32], F32)
        dg = sbf("dgA", [32, 32], F32)
        S.op("dve", lambda e: e.tensor_scalar(out=dg[:], in0=self.ident[0:32, 0:32], scalar1=hcol[:, 3:4], scalar2=None, op0=ALU.mult),
             reads=["ident", "hcol"], writes=["dgA"])
        S.op("pe", lambda e: e.matmul(ps[0][:, 0:32], ones[0:32, :], dg[:], start=True, stop=True), reads=["ones", "dgA"], writes=[("ps", 0)])
        S.op("act", lambda e: e.activation(out=abc[:], in_=ps[0][:, 0:32], func=AF.Copy), reads=[("ps", 0)], writes=["abc"])
        S.op("dve", lambda e: e.tensor_tensor(out=dA_tok2[:], in0=dt_tok[:], in1=abc[:].unsqueeze(1).broadcast_to([128, NT, 32]), op=ALU.mult),
             reads=["dt_tok", "abc"], writes=["dA_tok2"])
        dA_tok = dA_tok2

        for g in range(4):
            mg = self.scope_begin()
            Wz = sbf("Wz", [128, 8, 512], BF16)
            Wx = sbf("Wx", [128, 8, 512], BF16)
            WB = sbf("WB", [128, 8, 128], BF16)
            WC = sbf("WC", [128, 8, 128], BF16)
            Wout = sbf("Wout", [128, 4, 1024], BF16)
            nwb = sbf("nwb", [128, 512], F32)
            S.dma("sp", "d_const", nwb[:], self.nw_d[0:1, g * 512:(g + 1) * 512].partition_broadcast(128), writes=["nwb"])
            ms = self.scope_begin()
            self.stg = [sbf(f"stg{i}", [128, 2048], F32) for i in range(2)]
            self.stg_i = 0
            self.load_w(Wz, self.w_in[:, g * 512:(g + 1) * 512], "Wz", 8, 512)
            self.load_w(Wx, self.w_in[:, 2048 + g * 512:2048 + (g + 1) * 512], "Wx", 8, 512)
            self.load_w(WB, self.w_in[:, 4096 + g * 128:4096 + (g + 1) * 128], "WB", 8, 128)
            self.load_w(WC, self.w_in[:, 4608 + g * 128:4608 + (g + 1) * 128], "WC", 8, 128)
            self.load_w(Wout, self.w_out[g * 512:(g + 1) * 512, :], "Wout", 4, 1024)
            self.scope_end(ms)
            self.ssm_group(g, dict(Wz=Wz, Wx=Wx, WB=WB, WC=WC, Wout=Wout, nwb=nwb, identb=identb, uinc=uinc, ones=ones, negm=negm, cwT=cwT, cb=cb,
                                   dbc=dbc, dt_tok=dt_tok, nacs=nacs, ea=ea, dec=dec, dtdte=dtdte, dA_tok=dA_tok))
            self.scope_end(mg)
        self.scope_end(m0)

    def ssm_group(self, g, T):
        S = self.S
        ps = self.ps
        sbf = self.sb
        Wz, Wx, WB, WC, Wout, nwb, identb, uinc, ones, negm = (T[k] for k in ("Wz", "Wx", "WB", "WC", "Wout", "nwb", "identb", "uinc", "ones", "negm"))
        cwT, cb, dbc, dt_tok, nacs, ea, dec, dtdte, dA_tok = (T[k] for k in ("cwT", "cb", "dbc", "dt_tok", "nacs", "ea", "dec", "dtdte", "dA_tok"))
        xsT = sbf("xsT", [128, 4, 512], BF16)
        BT = sbf("BT", [128, 512], BF16)
        CT = [sbf(f"CT{i}", [128, 512], BF16) for i in range(2)]
        ub = [sbf(f"ub{i}", [128, 515], F32) for i in range(2)]
        acc = [sbf(f"acc{i}", [128, 512], F32) for i in range(2)]
        halo = sbf("halo", [128, 6, 3], F32)
        S.op("dve", lambda e: e.memset(halo[:], 0.0), writes=["halo"])
        dAU = sbf("dAU", [128, 8, 128], F32)
        DT = sbf("DT", [128, 8, 128], F32)
        MT = sbf("MT", [128, 8, 128], BF16)
        CBm = sbf("CBm", [128, 128], F32)
        nmb = sbf("nmb", [128, 8, 128], F32)
        Xt = sbf("Xt", [128, 8, 64], BF16)
        xsD = sbf("xsD", [128, 8, 64], BF16)
        Xd = [sbf(f"Xd{i}", [128, 8, 64], BF16) for i in range(2)]
        Btok = [sbf(f"Btok{i}", [128, 128], BF16) for i in range(2)]
        sz = [sbf(f"sz{i}", [128, 512], F32) for i in range(2)]
        S32 = sbf("S32", [128, 8, 64], F32)
        Sbf = sbf("Sbf", [128, 8, 64], BF16)
        Y = [sbf(f"Y{i}", [128, 8, 64], F32) for i in range(2)]
        sq = sbf("sq", [128, 512], F32)
        gn = sbf("gn", [128, 512], BF16)
        gnT = sbf("gnT", [128, 4, 128], BF16)
        S.op("dve", lambda e: e.memset(S32[:], 0.0), writes=["S32"])
        pyo = self.psT[:, 0:512]
        pcs = self.psT[:, 0:512]

        def phase_a(ct):
            sl = slice(ct * 512, (ct + 1) * 512)
            hk = [("hT", 4 * ct + i) for i in range(4)]
            ctb = CT[ct % 2]
            chunks_in = [(4 * g + i, ("xsT", i)) for i in range(4)] + [(16 + g, "BT"), (20 + g, ("CT", ct % 2))]
            for ci, (cidx, okey) in enumerate(chunks_in):
                q = ci % 2
                pp = ps[2 + q]
                if ci < 4:
                    W, wk, col = Wx, "Wx", slice(ci * 128, (ci + 1) * 128)
                elif ci == 4:
                    W, wk, col = WB, "WB", slice(0, 128)
                else:
                    W, wk, col = WC, "WC", slice(0, 128)
                self.mm_group(pp[:], [(W[:, kc, col], self.hT[:, kc, sl]) for kc in range(8)], reads=hk + [wk], writes=[("ps", 2 + q)])
                u, uk, a, ak = ub[q], ("ub", q), acc[q], ("acc", q)
                S.op("act", lambda e, u=u, ci=ci: e.activation(out=u[:, 0:3], in_=halo[:, ci, :], func=AF.Copy), reads=["halo"], writes=[uk])
                S.op("act", lambda e, u=u, pp=pp: e.activation(out=u[:, 3:515], in_=pp[:], func=AF.Copy), reads=[("ps", 2 + q)], writes=[uk])
                S.op("act", lambda e, u=u, ci=ci: e.activation(out=halo[:, ci, :], in_=u[:, 512:515], func=AF.Copy), reads=[uk], writes=["halo"])
                S.op("dve", lambda e, a=a, u=u, cidx=cidx: e.tensor_scalar(out=a[:], in0=u[:, 3:515], scalar1=cwT[:, cidx, 3:4], scalar2=cb[:, cidx:cidx + 1],
                                                                         op0=ALU.mult, op1=ALU.add), reads=[uk, "cwT", "cb"], writes=[ak])
                for w in (2, 1, 0):
                    S.op("dve", lambda e, a=a, u=u, cidx=cidx, w=w: e.scalar_tensor_tensor(out=a[:], in0=u[:, w:w + 512], scalar=cwT[:, cidx, w:w + 1], in1=a[:],
                                                                                      op0=ALU.mult, op1=ALU.add), reads=[uk, "cwT", ak], writes=[ak])
                dst = xsT[:, ci, :] if ci < 4 else (BT[:] if ci == 4 else ctb[:])
                S.op("act", lambda e, a=a, dst=dst: e.activation(out=dst, in_=a[:], func=AF.Silu), reads=[ak], writes=[okey])

        def stage1(c):
            ct, cq = divmod(c, 4)
            cs = slice(cq * 128, (cq + 1) * 128)
            hs = slice(8 * g, 8 * g + 8)
            par = c % 2
            ctb, ctk = CT[ct % 2], ("CT", ct % 2)
            xk = [("xsT", i) for i in range(4)]
            yb, ybk = ps[par], ("ps", par)
            bc = lambda t: t[:, c, hs].unsqueeze(2).broadcast_to([128, 8, 64])
            abank = [(ps[4][:], ("ps", 4)), (self.psT[:, 512:1024], "psT2")]
            S.op("dve", lambda e: e.tensor_tensor(out=dAU[:], in0=uinc[:].unsqueeze(1).broadcast_to([128, 8, 128]),
                                                  in1=dA_tok[:, c, hs].unsqueeze(2).broadcast_to([128, 8, 128]), op=ALU.mult),
                 reads=["uinc", "dA_tok2"], writes=["dAU"])
            S.op("dve", lambda e: e.tensor_tensor(out=nmb[:], in0=negm[:].rearrange("p (h l) -> p h l", h=4)[:, 0:1, :].broadcast_to([128, 8, 128]),
                                                  in1=nacs[:, c, hs].unsqueeze(2).broadcast_to([128, 8, 128]), op=ALU.add),
                 reads=["negm", "nacs"], writes=["nmb"])
            self.mm_group(ps[3][:], [(self.hT[:, kc, c * 128:(c + 1) * 128], Wz[:, kc, :]) for kc in range(8)],
                          reads=[("hT", c), "Wz"], writes=[("ps", 3)])
            S.op("act", lambda e: e.activation(out=sz[par][:], in_=ps[3][:], func=AF.Silu), reads=[("ps", 3)], writes=[("sz", par)])
            for hh in range(2):
                pb, pbk = abank[hh]
                S.op("pe", lambda e, hh=hh, pb=pb: e.matmul(pb, ones[:], dAU[:, 4 * hh:4 * hh + 4, :], start=True, stop=False),
                     reads=["ones", "dAU"], writes=[pbk], inc=False)
                S.op("pe", lambda e, hh=hh, pb=pb: e.matmul(pb, self.ident[:], nmb[:, 4 * hh:4 * hh + 4, :], start=False, stop=True),
                     reads=["ident", "nmb"], writes=[pbk])
                S.op("act", lambda e, hh=hh, pb=pb: e.activation(out=DT[:, 4 * hh:4 * hh + 4, :].rearrange("p h l -> p (h l)"), in_=pb, func=AF.Exp),
                     reads=[pbk], writes=[("DT", hh)])
            for i in range(4):
                S.op("pe", lambda e, i=i: e.matmul(ps[2][:, i * 128:(i + 1) * 128], xsT[:, i, cs], identb[:], start=True, stop=True),
                     reads=xk + ["identb"], writes=[("ps", 2)], inc=(i == 3))
            S.op("pe", lambda e: e.matmul(ps[3][:, 0:128], BT[:, cs], identb[:], start=True, stop=True), reads=["BT", "identb"], writes=[("ps", 3)], inc=False)
            S.op("pe", lambda e: e.matmul(ps[3][:, 128:256], BT[:, cs], ctb[:, cs], start=True, stop=True), reads=["BT", ctk], writes=[("ps", 3)])
            xv = ps[2][:].rearrange("p (h d) -> p h d", h=8)
            S.op("dve", lambda e: e.tensor_tensor(out=CBm[:], in0=ps[3][:, 128:256], in1=uinc[:], op=ALU.mult), reads=[("ps", 3), "uinc"], writes=["CBm"])
            S.op("act", lambda e: e.activation(out=Btok[par][:], in_=ps[3][:, 0:128], func=AF.Copy), reads=[("ps", 3)], writes=[("Btok", par)])
            for hh in range(2):
                S.op("dve", lambda e, hh=hh: e.tensor_tensor(out=MT[:, 4 * hh:4 * hh + 4, :], in0=DT[:, 4 * hh:4 * hh + 4, :],
                                                             in1=CBm[:].unsqueeze(1).broadcast_to([128, 4, 128]), op=ALU.mult),
                     reads=[("DT", hh), "CBm"], writes=[("MT", hh)])
            S.op("dve", lambda e: e.tensor_tensor(out=Xt[:], in0=xv, in1=bc(dt_tok), op=ALU.mult), reads=[("ps", 2), "dt_tok"], writes=["Xt"])
            S.op("dve", lambda e: e.tensor_tensor(out=xsD[:], in0=xv, in1=dbc[:, hs].unsqueeze(2).broadcast_to([128, 8, 64]), op=ALU.mult),
                 reads=[("ps", 2), "dbc"], writes=["xsD"])
            S.op("dve", lambda e: e.tensor_tensor(out=Xd[par][:], in0=xv, in1=bc(dtdte), op=ALU.mult), reads=[("ps", 2), "dtdte"], writes=[("Xd", par)])
            S.op("pe", lambda e: e.matmul(yb[:], identb[:], xsD[:].rearrange("p h d -> p (h d)"), start=True, stop=False),
                 reads=["identb", "xsD"], writes=[ybk], inc=False)
            for hl in range(8):
                S.op("pe", lambda e, hl=hl: e.matmul(yb[:, hl * 64:(hl + 1) * 64], MT[:, hl, :], Xt[:, hl, :], start=False, stop=(hl == 7)),
                     reads=[("MT", hl // 4), "Xt"], writes=[ybk], inc=(hl == 7))

        def stage2a(c):
            ct, cq = divmod(c, 4)
            cs = slice(cq * 128, (cq + 1) * 128)
            hs = slice(8 * g, 8 * g + 8)
            par = c % 2
            ctb, ctk = CT[ct % 2], ("CT", ct % 2)
            yb, ybk = ps[par], ("ps", par)
            Yc, Yk = Y[par], ("Y", par)
            bc = lambda t: t[:, c, hs].unsqueeze(2).broadcast_to([128, 8, 64])
            Yf = Yc[:].rearrange("p h d -> p (h d)")
            if c > 0:
                S.op("pe", lambda e: e.matmul(pyo, ctb[:, cs], Sbf[:].rearrange("p h d -> p (h d)"), start=True, stop=True),
                     reads=[ctk, "Sbf"], writes=["psT"])
                S.op("dve", lambda e: e.tensor_tensor(out=Yc[:], in0=pyo.rearrange("p (h d) -> p h d", h=8), in1=bc(ea), op=ALU.mult),
                     reads=["psT", "ea"], writes=[Yk])
            if c < NT - 1:
                S.op("pe", lambda e: e.matmul(pcs, Btok[par][:], Xd[par][:].rearrange("p h d -> p (h d)"), start=True, stop=True),
                     reads=[("Btok", par), ("Xd", par)], writes=["psT"])
                S.op("dve", lambda e: e.tensor_tensor(out=S32[:], in0=S32[:], in1=bc(dec), op=ALU.mult), reads=["S32", "dec"], writes=["S32"])
                S.op("dve", lambda e: e.tensor_tensor(out=S32[:].rearrange("p h d -> p (h d)"), in0=pcs, in1=S32[:].rearrange("p h d -> p (h d)"), op=ALU.add),
                     reads=["psT", "S32"], writes=["S32"])
                S.op("act", lambda e: e.activation(out=Sbf[:], in_=S32[:], func=AF.Copy), reads=["S32"], writes=["Sbf"])
            if c > 0:
                S.op("dve", lambda e: e.tensor_tensor(out=Yf, in0=yb[:], in1=Yf, op=ALU.add), reads=[ybk, Yk], writes=[Yk])
            else:
                S.op("act", lambda e: e.activation(out=Yf, in_=yb[:], func=AF.Copy), reads=[ybk], writes=[Yk])

        def stage2b(c):
            par = c % 2
            Yc, Yk = Y[par], ("Y", par)
            Yf = Yc[:].rearrange("p h d -> p (h d)")
            S.op("dve", lambda e: e.tensor_tensor(out=Yf, in0=Yf, in1=sz[par][:], op=ALU.mult), reads=[Yk, ("sz", par)], writes=[Yk])
            sm = self.sm[par]
            smk = ("sm", par)
            S.op("act", lambda e: e.activation(out=sq[:], in_=Yf, func=AF.Square, accum_out=sm[:, 0:1]), reads=[Yk], writes=["sq", smk])
            S.op("act", lambda e: e.activation(out=sm[:, 1:2], in_=sm[:, 0:1], func=AF.Sqrt, scale=1.0 / 512, bias=self.epsb[:, 0:1]), reads=[smk, "epsb"], writes=[smk])
            S.op("dve", lambda e: e.reciprocal(out=sm[:, 2:3], in_=sm[:, 1:2]), reads=[smk], writes=[smk])
            S.op("dve", lambda e: e.scalar_tensor_tensor(out=gn[:], in0=Yf, scalar=sm[:, 2:3], in1=nwb[:], op0=ALU.mult, op1=ALU.mult),
                 reads=[Yk, smk, "nwb"], writes=["gn"])
            for i in range(4):
                S.op("pe", lambda e, i=i: e.matmul(ps[5][:, i * 128:(i + 1) * 128], gn[:, i * 128:(i + 1) * 128], identb[:], start=True, stop=True),
                     reads=["gn", "identb"], writes=[("ps", 5)], inc=(i == 3))
            S.op("act", lambda e: e.activation(out=gnT[:], in_=ps[5][:].rearrange("p (k t) -> p k t", k=4), func=AF.Copy), reads=[("ps", 5)], writes=["gnT"])
            for half in range(2):
                self.mm_group(ps[5][:], [(gnT[:, i, :], Wout[:, i, half * 512:(half + 1) * 512]) for i in range(4)],
                              reads=["gnT", "Wout"], writes=[("ps", 5)])
                xs_ = self.x[:, c, half * 512:(half + 1) * 512]
                S.op("dve", lambda e, xs_=xs_: e.tensor_tensor(out=xs_, in0=ps[5][:], in1=xs_, op=ALU.add), reads=[("ps", 5), ("x", c)], writes=[("x", c)])

        phase_a(0)
        S.emit_interleaved([S.record(lambda: stage1(0))])
        for c in range(NT + 1):
            lists = []
            if c < NT:
                lists.append(S.record(lambda c=c: stage2a(c)))
            if c >= 1:
                lists.append(S.record(lambda c=c: stage2b(c - 1)))
            if c + 1 < NT:
                if (c + 1) % 4 == 0:
                    phase_a((c + 1) // 4)
                lists.append(S.record(lambda c=c: stage1(c + 1)))
            S.emit_interleaved(lists)

    def router_tile(self, li, tt):
        S = self.S
        p = tt % 2
        sm = self.sm[p]
        k = ("sm", p)
        h32 = self.h32[p]
        psR = self.ps[4 + p]
        self.mm_group(psR[:, 0:72], [(h32[:, kc, :], self.wr[:, kc, :]) for kc in range(8)],
                      reads=[("h32", p), "wr"], writes=[("ps", 4 + p)])
        lg = sm[:, 8:80]
        V = lambda fn, r=(), w=(): S.op("dve", fn, reads=[k] + list(r), writes=[k] + list(w))
        V(lambda e: e.tensor_tensor(out=lg, in0=psR[:, 0:72], in1=self.rb[:], op=ALU.add), r=[("ps", 4 + p), "rb"])
        gmax, ngmax, gsum, gp = sm[:, 80:81], sm[:, 81:82], sm[:, 82:83], sm[:, 83:84]
        V(lambda e: e.tensor_reduce(out=gmax, in_=lg[:, 0:8], axis=AX.X, op=ALU.max))
        V(lambda e: e.tensor_scalar(out=ngmax, in0=gmax, scalar1=-1.0, scalar2=None, op0=ALU.mult))
        S.op("act", lambda e: e.activation(out=sm[:, 96:104], in_=lg[:, 0:8], func=AF.Exp, bias=ngmax, scale=1.0, accum_out=gsum),
             reads=[k], writes=[k])
        V(lambda e: e.reciprocal(out=gp, in_=gsum))
        gmask = sm[:, 104:112]
        V(lambda e: e.tensor_scalar(out=gmask, in0=lg[:, 0:8], scalar1=gmax, scalar2=None, op0=ALU.is_equal))
        negb = sm[:, 112:120]
        V(lambda e: e.tensor_scalar(out=negb, in0=gmask, scalar1=1.0, scalar2=1e30, op0=ALU.subtract, op1=ALU.mult))
        elm = sm[:, 128:192]
        V(lambda e: e.tensor_tensor(out=elm.rearrange("p (g j) -> p g j", g=8), in0=lg[:, 8:72].rearrange("p (g j) -> p g j", g=8),
                                    in1=negb.unsqueeze(2).broadcast_to([128, 8, 8]), op=ALU.add))
        m1, m2, dd, ed, s1, g1, g2 = (sm[:, 84 + i:85 + i] for i in range(7))
        V(lambda e: e.tensor_reduce(out=m1, in_=elm, axis=AX.X, op=ALU.max))
        mask1 = sm[:, 192:256]
        V(lambda e: e.tensor_scalar(out=mask1, in0=elm, scalar1=m1, scalar2=None, op0=ALU.is_equal))
        elm2 = sm[:, 256:320]
        V(lambda e: e.scalar_tensor_tensor(out=elm2, in0=mask1, scalar=-1e30, in1=elm, op0=ALU.mult, op1=ALU.add))
        V(lambda e: e.tensor_reduce(out=m2, in_=elm2, axis=AX.X, op=ALU.max))
        mask2 = sm[:, 320:384]
        V(lambda e: e.tensor_scalar(out=mask2, in0=elm2, scalar1=m2, scalar2=None, op0=ALU.is_equal))
        V(lambda e: e.tensor_tensor(out=dd, in0=m2, in1=m1, op=ALU.subtract))
        S.op("act", lambda e: e.activation(out=ed, in_=dd, func=AF.Exp), reads=[k], writes=[k])
        V(lambda e: e.tensor_scalar(out=ed, in0=ed, scalar1=1.0, scalar2=None, op0=ALU.add))
        V(lambda e: e.reciprocal(out=s1, in_=ed))
        V(lambda e: e.tensor_tensor(out=g1, in0=gp, in1=s1, op=ALU.mult))
        V(lambda e: e.tensor_tensor(out=g2, in0=gp, in1=g1, op=ALU.subtract))
        V(lambda e: e.tensor_scalar(out=mask1, in0=mask1, scalar1=g1, scalar2=None, op0=ALU.mult))
        V(lambda e: e.scalar_tensor_tensor(out=self.G[:, tt, :], in0=mask2, scalar=g2, in1=mask1, op0=ALU.mult, op1=ALU.add),
          w=[("G", tt)])

    def router_batched(self, LG):
        S = self.S
        sbf = self.sb
        B4 = [128, NT, 8, 8]
        B3 = [128, NT, 8]
        gl = LG[:, :, 0:8]
        el = LG[:, :, 8:72].rearrange("p t (g j) -> p t g j", g=8)
        t16 = sbf("r_t16", [128, 12, NT], F32)
        gmax, gsum, gp, m1, m2, dd, ed, s1, g1, g2 = (t16[:, i, :] for i in range(10))
        gsh = sbf("r_gsh", B3, F32)
        gex = sbf("r_gex", B3, F32)
        gmask = sbf("r_gmask", B3, F32)
        elm = sbf("r_elm", [128, NT, 64], F32)
        mask1 = sbf("r_mask1", [128, NT, 64], F32)
        elm2 = sbf("r_elm2", [128, NT, 64], F32)
        mask2 = sbf("r_mask2", [128, NT, 64], F32)
        k = "rtr"
        V = lambda fn, r=(), w=(): S.op("dve", fn, reads=[k] + list(r), writes=[k] + list(w))
        A = lambda fn: S.op("act", fn, reads=[k], writes=[k])
        b3 = lambda v: v.unsqueeze(2).broadcast_to(B3)
        b64 = lambda v: v.unsqueeze(2).broadcast_to([128, NT, 64])
        f2 = lambda t: t[:].rearrange("p t e -> p (t e)")
        lgk = [("LG", tt) for tt in range(NT)]
        V(lambda e: e.tensor_reduce(out=gmax, in_=gl, axis=AX.X, op=ALU.max), r=lgk)
        V(lambda e: e.tensor_tensor(out=gsh[:], in0=gl, in1=b3(gmax), op=ALU.subtract), r=lgk)
        A(lambda e: e.activation(out=gex[:], in_=gsh[:], func=AF.Exp))
        V(lambda e: e.tensor_reduce(out=gsum, in_=gex[:], axis=AX.X, op=ALU.add))
        V(lambda e: e.reciprocal(out=gp, in_=gsum))
        V(lambda e: e.tensor_scalar(out=gmask[:], in0=gsh[:], scalar1=0.0, scalar2=None, op0=ALU.is_equal))
        V(lambda e: e.tensor_scalar(out=gmask[:], in0=gmask[:], scalar1=1.0, scalar2=1e30, op0=ALU.subtract, op1=ALU.mult))
        V(lambda e: e.tensor_tensor(out=elm[:].rearrange("p t (g j) -> p t g j", g=8), in0=el, in1=gmask[:].unsqueeze(3).broadcast_to(B4), op=ALU.add), r=lgk)
        V(lambda e: e.tensor_reduce(out=m1, in_=elm[:], axis=AX.X, op=ALU.max))
        V(lambda e: e.tensor_tensor(out=mask1[:], in0=elm[:], in1=b64(m1), op=ALU.is_equal))
        V(lambda e: e.scalar_tensor_tensor(out=f2(elm2), in0=f2(mask1), scalar=-1e30, in1=f2(elm), op0=ALU.mult, op1=ALU.add))
        V(lambda e: e.tensor_reduce(out=m2, in_=elm2[:], axis=AX.X, op=ALU.max))
        V(lambda e: e.tensor_tensor(out=mask2[:], in0=elm2[:], in1=b64(m2), op=ALU.is_equal))
        V(lambda e: e.tensor_tensor(out=dd, in0=m2, in1=m1, op=ALU.subtract))
        A(lambda e: e.activation(out=ed, in_=dd, func=AF.Exp))
        V(lambda e: e.tensor_scalar(out=ed, in0=ed, scalar1=1.0, scalar2=None, op0=ALU.add))
        V(lambda e: e.reciprocal(out=s1, in_=ed))
        V(lambda e: e.tensor_tensor(out=g1, in0=gp, in1=s1, op=ALU.mult))
        V(lambda e: e.tensor_tensor(out=g2, in0=gp, in1=g1, op=ALU.subtract))
        V(lambda e: e.tensor_tensor(out=mask1[:], in0=mask1[:], in1=b64(g1), op=ALU.mult))
        V(lambda e: e.tensor_tensor(out=mask2[:], in0=mask2[:], in1=b64(g2), op=ALU.mult))
        V(lambda e: e.tensor_tensor(out=self.G[:], in0=mask1[:], in1=mask2[:], op=ALU.add), w=[("G", tt) for tt in range(NT)])

    def load_expert(self, li, e, slot):
        S = self.S
        pieces = []
        for nm, w, dst in (("Wg", self.w_gate, self.Wg[slot]), ("Wu", self.w_up, self.Wu[slot])):
            for h in range(2):
                src = w[li, e, h * 512:(h + 1) * 512, :].rearrange("(kc p) f -> p kc f", p=128)
                pieces.append((src, dst[:, h * 4:(h + 1) * 4, :], (nm, slot), [128, 4, 512]))
        for h in range(2):
            src = self.w_down[li, e, h * 256:(h + 1) * 256, :].rearrange("(kc p) f -> p kc f", p=128)
            pieces.append((src, self.Wd[slot][:, h * 2:(h + 1) * 2, :], ("Wd", slot), [128, 2, 1024]))
        for src, dst, key, shp in pieces:
            si = self.stg_i % 2
            self.stg_i += 1
            stg = self.stg[si]
            view = stg[:].rearrange("p (a b) -> p a b", a=shp[1])
            S.dma("sp", f"d_stg{si}", view, src, writes=[("stg", si)])
            S.op("pool", lambda en, dst=dst, view=view: en.tensor_copy(out=dst, in_=view), reads=[("stg", si)], writes=[key])

    def moe_phase(self, li):
        S = self.S
        m0 = self.scope_begin()
        self.G = self.sb("G", [128, NT, NE], F32)
        m1 = self.scope_begin()
        self.load_lnw(self.ln_ffn[li:li + 1, :])
        self.wr = self.sb("wr", [128, 8, 72], F32)
        self.rb = self.sb("rb", [128, 72], F32)
        S.dma("sp", "d_const", self.wr[:], self.w_router[li].rearrange("(kc p) e -> p kc e", p=128), writes=["wr"])
        S.dma("sp", "d_const", self.rb[:], self.b_router[li:li + 1, :].partition_broadcast(128), writes=["rb"])
        self.h32 = [self.sb(f"h32_{i}", [128, 8, 128], F32) for i in range(2)]
        LG = self.sb("LG", [128, NT, 72], F32)
        def rt(tt):
            p = tt % 2
            self.norm_tile(tt, want32=(self.h32[p], ("h32", p)))
            psR = self.ps[4 + p]
            self.mm_group(psR[:, 0:72], [(self.h32[p][:, kc, :], self.wr[:, kc, :]) for kc in range(8)],
                          reads=[("h32", p), "wr"], writes=[("ps", 4 + p)])
            S.op("dve", lambda e: e.tensor_tensor(out=LG[:, tt, :], in0=psR[:, 0:72], in1=self.rb[:], op=ALU.add),
                 reads=[("ps", 4 + p), "rb"], writes=[("LG", tt)])
        self.norm_all(rt)
        self.router_batched(LG)
        self.scope_end(m1)
        self.Wg = [self.sb(f"Wg{i}", [128, 8, FF], BF16) for i in range(2)]
        self.Wu = [self.sb(f"Wu{i}", [128, 8, FF], BF16) for i in range(2)]
        self.Wd = [self.sb(f"Wd{i}", [128, 4, D], BF16) for i in range(2)]
        self.stg = [self.sb(f"stg{i}", [128, 2048], F32) for i in range(2)]
        self.sg = [self.sb(f"sg{i}", [128, 512], BF16) for i in range(2)]
        self.hid = [self.sb(f"hid{i}", [128, 4, 512], BF16) for i in range(2)]
        self.stg_i = 0
        nE = self.n_exp
        obanks = [(self.ps[4][:], ("ps", 4)), (self.ps[5][:], ("ps", 5)), (self.psT[:, 0:512], "psT"), (self.psT[:, 512:1024], "psT2")]

        def gate_up(e, ct):
            slot = e % 2
            Wg, Wu = self.Wg[slot], self.Wu[slot]
            hid = self.hid[ct % 2]
            hkeys = [("hT", 4 * ct + i) for i in range(4)]
            rhs = [self.hT[:, kc, ct * 512:(ct + 1) * 512] for kc in range(8)]
            for fc in range(4):
                q = fc % 2
                pg, pu = self.ps[q], self.ps[2 + q]
                self.mm_group(pg[:], [(Wg[:, kc, fc * 128:(fc + 1) * 128], rhs[kc]) for kc in range(8)], reads=hkeys + [("Wg", slot)], writes=[("ps", q)])
                self.mm_group(pu[:], [(Wu[:, kc, fc * 128:(fc + 1) * 128], rhs[kc]) for kc in range(8)], reads=hkeys + [("Wu", slot)], writes=[("ps", 2 + q)])
                S.op("act", lambda en, q=q, pg=pg: en.activation(out=self.sg[q][:], in_=pg[:], func=AF.Silu), reads=[("ps", q)], writes=[("sg", q)])
                S.op("dve", lambda en, q=q, pu=pu, fc=fc: en.tensor_tensor(out=hid[:, fc, :], in0=pu[:], in1=self.sg[q][:], op=ALU.mult),
                     reads=[("ps", 2 + q), ("sg", q)], writes=[("hid", ct % 2, fc)])

        def down(e, ct):
            slot = e % 2
            Wd = self.Wd[slot]
            hid = self.hid[ct % 2]
            for tq in range(4):
                tt = ct * 4 + tq
                for half in range(2):
                    po, pk = obanks[(tq * 2 + half) % 4]
                    self.mm_group(po, [(hid[:, fc, tq * 128:(tq + 1) * 128], Wd[:, fc, half * 512:(half + 1) * 512]) for fc in range(4)],
                                  reads=[("hid", ct % 2, fc) for fc in range(4)] + [("Wd", slot)], writes=[pk])
                    xs = self.x[:, tt, half * 512:(half + 1) * 512]
                    S.op("dve", lambda en, po=po, xs=xs, tt=tt: en.scalar_tensor_tensor(
                        out=xs, in0=po, scalar=self.G[:, tt, e:e + 1], in1=xs, op0=ALU.mult, op1=ALU.add),
                         reads=[pk, ("G", tt), ("xh", tt, half)], writes=[("xh", tt, half)])

        units = [(e, ct) for e in range(nE) for ct in range(4)]
        self.load_expert(li, 0, 0)
        if nE > 1:
            self.load_expert(li, 1, 1)
        gate_up(*units[0])
        for i, (e, ct) in enumerate(units):
            if i + 1 < len(units):
                ne, nct = units[i + 1]
                gate_up(ne, nct)
            down(e, ct)
            if ct == 3 and e + 2 < nE:
                self.load_expert(li, e + 2, e % 2)
        self.scope_end(m0)

    def final_phase(self, s):
        S = self.S
        xo = self.out[s].rearrange("(tt p) d -> p tt d", p=128)
        mf = self.scope_begin()
        self.load_lnw(self.ln_final[0:1, :])
        for tt in range(NT):
            p = tt % 2
            sm = self.sm[p]
            hn = self.hn[p]
            xk = ("x", tt)
            S.op("act", lambda e: e.activation(out=hn[:], in_=self.x[:, tt, :], func=AF.Square, accum_out=sm[:, 0:1]),
                 reads=[xk], writes=[("sm", p), ("hn", p)])
            S.op("act", lambda e: e.activation(out=sm[:, 1:2], in_=sm[:, 0:1], func=AF.Sqrt, scale=1.0 / D, bias=self.epsb[:, 0:1]),
                 reads=[("sm", p), "epsb"], writes=[("sm", p)])
            S.op("dve", lambda e: e.reciprocal(out=sm[:, 2:3], in_=sm[:, 1:2]), reads=[("sm", p)], writes=[("sm", p)])
            S.op("dve", lambda e: e.scalar_tensor_tensor(out=hn[:], in0=self.x[:, tt, :], scalar=sm[:, 2:3], in1=self.lnw[:],
                                                         op0=ALU.mult, op1=ALU.mult),
                 reads=[xk, ("sm", p), "lnw"], writes=[("hn", p)])
            S.dma("sp", f"d_o{p}", xo[:, tt, :], hn[:], reads=[("hn", p)], writes=[f"fin{p}"])
        self.scope_end(mf)


def _consts():
    return {"ident": np.eye(128, dtype=np.float32)}


def _bucket_table():
    n = np.arange(128)
    nf = np.maximum(n, 1).astype(np.float32)
    large = 16 + (np.log(nf / np.float32(16)) / np.float32(np.log(128 / 16)) * np.float32(16)).astype(np.int32)
    large = np.minimum(large, 31)
    return np.where(n < 16, n, large)


def _bias_tables(rel_bias):
    bt = _bucket_table()
    k = np.arange(128)[:, None]
    q = np.arange(128)[None, :]
    rb = np.asarray(rel_bias, dtype=np.float32)
    dc = q - k
    vc = dc >= 0
    tc = rb[bt[np.where(vc, dc, 0)]]
    tc = np.where(vc[:, :, None], tc, np.float32(NEG))
    dp = q + 128 - k
    vp = dp < 128
    tp = rb[bt[np.where(vp, dp, 0)]]
    tp = np.where(vp[:, :, None], tp, np.float32(NEG))
    return (np.ascontiguousarray(tc.transpose(0, 2, 1), dtype=np.float32),
            np.ascontiguousarray(tp.transpose(0, 2, 1), dtype=np.float32))


def make_in_maps(inputs, n_seq=2, n_cores=8, phases=("attn", "moe0", "ssm", "moe1", "final")):
    x = np.ascontiguousarray(inputs["x"], dtype=np.float32)
    w_router = np.ascontiguousarray(np.concatenate([inputs["moe_w_group"], inputs["moe_w_expert"]], axis=2), dtype=np.float32)
    b_router = np.ascontiguousarray(np.concatenate([inputs["moe_b_group"], inputs["moe_b_expert"]], axis=1), dtype=np.float32)
    shared = {
        "ln_mix": inputs["ln_mix"], "ln_ffn": inputs["ln_ffn"], "ln_final": np.asarray(inputs["ln_final"]).reshape(1, D),
        "w_router": w_router, "b_router": b_router,
        "moe_w_gate": inputs["moe_w_gate"], "moe_w_up": inputs["moe_w_up"], "moe_w_down": inputs["moe_w_down"],
    }
    if "attn_w_qkv" in inputs and "attn" in phases:
        tc, tp = _bias_tables(inputs["rel_bias"])
        shared.update({"attn_w_qkv": np.asarray(inputs["attn_w_qkv"]).reshape(D, 1536), "attn_w_o": np.asarray(inputs["attn_w_o"]).reshape(D, D),
                       "attn_sinks": np.asarray(inputs["attn_sinks"]).reshape(1, 16), "tcur": tc, "tprev": tp})
    if "ssm_w_in" in inputs and "ssm" in phases:
        cw = np.asarray(inputs["ssm_conv_w"], dtype=np.float32).reshape(4, 3072)
        k_ = np.arange(128)[:, None]
        l_ = np.arange(128)[None, :]
        shared.update({
            "ssm_w_in": np.asarray(inputs["ssm_w_in"]).reshape(D, 5152), "ssm_w_out": np.asarray(inputs["ssm_w_out"]).reshape(2048, D),
            "ssm_cwT": cw.T.reshape(24, 128, 4).transpose(1, 0, 2),
            "ssm_cb": np.asarray(inputs["ssm_conv_b"], dtype=np.float32).reshape(24, 128).T,
            "ssm_hcol": np.stack([np.asarray(inputs["ssm_dt_bias"]).reshape(32), np.asarray(inputs["ssm_a_log"]).reshape(32)], axis=1),
            "ssm_d": np.asarray(inputs["ssm_d"]).reshape(1, 32), "ssm_norm_w": np.asarray(inputs["ssm_norm_w"]).reshape(1, 2048),
            "c_uincl": (k_ <= l_).astype(np.float32), "c_ones": np.ones((128, 128), np.float32),
            "c_negmask4": np.tile(np.where(k_ > l_, np.float32(NEG), np.float32(0.0)), (1, 4)),
        })
    shared = {k: np.ascontiguousarray(v, dtype=np.float32) for k, v in shared.items()}
    shared.update(_consts())
    maps = []
    for c in range(n_cores):
        m = dict(shared)
        m["x"] = x[c * n_seq:(c + 1) * n_seq]
        maps.append(m)
    return maps


def kernel(**inputs):
    kb = K()
    nc = kb.build()
    in_maps = make_in_maps(inputs)
    res = run_bass_kernel_spmd(nc, in_maps, core_ids=list(range(8)))
    return np.concatenate([r["out"] for r in res.results], axis=0).astype(np.float32)
```

```python
import numpy as np
import concourse.bass as bass
import concourse.mybir as mybir
from concourse.bass_utils import run_bass_kernel_spmd

F32 = mybir.dt.float32
BF16 = mybir.dt.bfloat16
AF = mybir.ActivationFunctionType
ALU = mybir.AluOpType
AX = mybir.AxisListType

D = 1024
L = 2048
NT = 16
NE = 64
FF = 512
EPS = 1e-6
NEG = -30000.0


class Sched:
    def __init__(self, nc, self_sync=True):
        self.nc = nc
        self.eng = {"pe": nc.tensor, "act": nc.scalar, "dve": nc.vector, "pool": nc.gpsimd, "sp": nc.sync}
        self.sem = {}
        self.cnt = {}
        self.seen = {k: {} for k in self.eng}
        self.last_w = {}
        self.readers = {}
        self.self_sync = self_sync
        self._ctx = []
        self.n_ins = 0
        for k in self.eng:
            self._mk(k)

    def _mk(self, name):
        cm = self.nc.semaphore("s_" + name)
        s = cm.__enter__()
        self._ctx.append(cm)
        self.sem[name] = s
        self.cnt[name] = 0

    @staticmethod
    def _is_psum(k):
        return k in ("psT", "psT2") or (isinstance(k, tuple) and k[0] == "ps")

    def _deps(self, reads, writes, en=None):
        deps = {}
        for k in reads:
            t = self.last_w.get(k)
            if t is not None:
                deps[t[0]] = max(deps.get(t[0], 0), t[1])
            if self._is_psum(k):
                for s, i in self.readers.get(k, {}).items():
                    if s != en:
                        deps[s] = max(deps.get(s, 0), i)
        for k in writes:
            t = self.last_w.get(k)
            if t is not None:
                deps[t[0]] = max(deps.get(t[0], 0), t[1])
            for s, i in self.readers.get(k, {}).items():
                deps[s] = max(deps.get(s, 0), i)
        return deps

    def _wait(self, en, deps):
        e = self.eng[en]
        for src, idx in deps.items():
            if src == en and (en == "pe" or not self.self_sync):
                continue
            if self.seen[en].get(src, 0) >= idx:
                continue
            e.wait_ge(self.sem[src], idx)
            self.seen[en][src] = idx

    def _record(self, tag, reads, writes):
        for k in reads:
            r = self.readers.setdefault(k, {})
            r[tag[0]] = max(r.get(tag[0], 0), tag[1])
        for k in writes:
            self.last_w[k] = tag
            self.readers[k] = {}

    def record(self, f):
        self.rec = []
        f()
        lst, self.rec = self.rec, None
        return lst

    def emit_interleaved(self, lists):
        its = [list(l) for l in lists if l]
        pos = [0] * len(its)
        while any(p < len(l) for p, l in zip(pos, its)):
            for i, l in enumerate(its):
                while pos[i] < len(l):
                    o = l[pos[i]]
                    pos[i] += 1
                    if o[0] == "op":
                        self.op(*o[1:])
                        if not (o[1] == "pe" and o[5] is False):
                            break
                    else:
                        self.dma(*o[1:])
                        break

    def op(self, en, fn, reads=(), writes=(), inc=True):
        if getattr(self, "rec", None) is not None:
            self.rec.append(("op", en, fn, tuple(reads), tuple(writes), inc))
            return None
        self._wait(en, self._deps(reads, writes, en))
        ins = fn(self.eng[en])
        self.n_ins += 1
        if inc:
            ins.then_inc(self.sem[en], 1)
            self.cnt[en] += 1
            tag = (en, self.cnt[en])
        else:
            tag = (en, self.cnt[en] + 1)
        self._record(tag, reads, writes)
        return ins

    def dma(self, qn, dsem, out, in_, reads=(), writes=()):
        if getattr(self, "rec", None) is not None:
            self.rec.append(("dma", qn, dsem, out, in_, tuple(reads), tuple(writes)))
            return None
        if dsem not in self.sem:
            self._mk(dsem)
        deps = self._deps(reads, writes)
        if self.cnt[dsem] > 0:
            deps[dsem] = max(deps.get(dsem, 0), self.cnt[dsem])
        self._wait(qn, deps)
        ins = self.eng[qn].dma_start(out=out, in_=in_)
        ins.then_inc(self.sem[dsem], 16)
        self.n_ins += 1
        self.cnt[dsem] += 16
        self._record((dsem, self.cnt[dsem]), reads, writes)
        return ins

    def barrier(self):
        for en in self.eng:
            deps = {src: c for src, c in self.cnt.items() if c > 0}
            self._wait(en, deps)

    def wait_keys(self, en, keys):
        deps = {}
        for k in keys:
            t = self.last_w.get(k)
            if t is not None:
                deps[t[0]] = max(deps.get(t[0], 0), t[1])
            for s, i in self.readers.get(k, {}).items():
                deps[s] = max(deps.get(s, 0), i)
        self._wait(en, deps)


class K:
    def __init__(self, n_seq=2, phases=("attn", "moe0", "ssm", "moe1", "final"), n_exp=NE, debug_x=False):
        self.n_seq = n_seq
        self.phases = phases
        self.n_exp = n_exp
        nc = self.nc = bass.Bass("TRN2", target_bir_lowering=False)
        self.S = Sched(nc)
        self._cms = []
        dt = nc.dram_tensor
        self.x_in = dt("x", [n_seq, L, D], F32, kind="ExternalInput").ap()
        self.out = dt("out", [n_seq, L, D], F32, kind="ExternalOutput").ap()
        self.ln_mix = dt("ln_mix", [2, D], F32, kind="ExternalInput").ap()
        self.ln_ffn = dt("ln_ffn", [2, D], F32, kind="ExternalInput").ap()
        self.ln_final = dt("ln_final", [1, D], F32, kind="ExternalInput").ap()
        self.ident_d = dt("ident", [128, 128], F32, kind="ExternalInput").ap()
        if "attn" in phases:
          self.w_qkv = dt("attn_w_qkv", [D, 1536], F32, kind="ExternalInput").ap()
          self.w_o = dt("attn_w_o", [D, D], F32, kind="ExternalInput").ap()
          self.sinks = dt("attn_sinks", [1, 16], F32, kind="ExternalInput").ap()
          self.tcur_d = dt("tcur", [128, 16, 128], F32, kind="ExternalInput").ap()
          self.tprev_d = dt("tprev", [128, 16, 128], F32, kind="ExternalInput").ap()
        if "ssm" in phases:
          self.w_in = dt("ssm_w_in", [D, 5152], F32, kind="ExternalInput").ap()
          self.w_out = dt("ssm_w_out", [2048, D], F32, kind="ExternalInput").ap()
          self.cwT_d = dt("ssm_cwT", [128, 24, 4], F32, kind="ExternalInput").ap()
          self.cb_d = dt("ssm_cb", [128, 24], F32, kind="ExternalInput").ap()
          self.hcol_d = dt("ssm_hcol", [32, 2], F32, kind="ExternalInput").ap()
          self.dsk_d = dt("ssm_d", [1, 32], F32, kind="ExternalInput").ap()
          self.nw_d = dt("ssm_norm_w", [1, 2048], F32, kind="ExternalInput").ap()
          self.uinc_d = dt("c_uincl", [128, 128], F32, kind="ExternalInput").ap()
          self.ones_d = dt("c_ones", [128, 128], F32, kind="ExternalInput").ap()
          self.negm_d = dt("c_negmask4", [128, 512], F32, kind="ExternalInput").ap()
        self.w_router = dt("w_router", [2, D, 72], F32, kind="ExternalInput").ap()
        self.b_router = dt("b_router", [2, 72], F32, kind="ExternalInput").ap()
        self.w_gate = dt("moe_w_gate", [2, n_exp, D, FF], F32, kind="ExternalInput").ap()
        self.w_up = dt("moe_w_up", [2, n_exp, D, FF], F32, kind="ExternalInput").ap()
        self.w_down = dt("moe_w_down", [2, n_exp, FF, D], F32, kind="ExternalInput").ap()

    def sb(self, name, shape, dtype=F32):
        self._uid = getattr(self, "_uid", 0) + 1
        cm = self.nc.sbuf_tensor(f"{name}_{self._uid}", shape, dtype)
        t = cm.__enter__()
        self._cms.append(cm)
        return t

    def pst(self, name, shape, dtype=F32):
        cm = self.nc.psum_tensor(name, shape, dtype)
        t = cm.__enter__()
        self._cms.append(cm)
        return t

    def scope_begin(self):
        return len(self._cms)

    def scope_end(self, mark):
        self.S.barrier()
        while len(self._cms) > mark:
            self._cms.pop().__exit__(None, None, None)

    def load_lnw(self, src_row):
        self.lnw = self.sb("lnw", [128, D], F32)
        self.hn = [self.sb(f"hn{i}", [128, D], F32) for i in range(2)]
        self.S.dma("sp", "d_const", self.lnw[:], src_row.partition_broadcast(128), writes=["lnw"])

    def norm_all(self, per_tile):
        for t0 in range(0, NT, 2):
            recs = [self.S.record(lambda tt=tt: per_tile(tt)) for tt in (t0, t0 + 1)]
            self.S.emit_interleaved(recs)

    def mm_group(self, out, pairs, reads, writes):
        n = len(pairs)
        for i, (lhsT, rhs) in enumerate(pairs):
            self.S.op("pe", lambda e, lhsT=lhsT, rhs=rhs, i=i: e.matmul(out, lhsT, rhs, start=(i == 0), stop=(i == n - 1)),
                      reads=reads, writes=writes, inc=(i == n - 1))

    def build(self):
        S = self.S
        nc = self.nc
        self.x = self.sb("xres", [128, NT, D], F32)
        self.hT = self.sb("hT", [128, 8, L], BF16)
        self.ident = self.sb("ident_sb", [128, 128], F32)
        self.sm = [self.sb(f"sm{i}", [128, 512], F32) for i in range(2)]
        self.epsb = self.sb("epsb", [128, 1], F32)
        S.op("dve", lambda e: e.memset(self.epsb[:], EPS), writes=["epsb"])
        self.oneb = self.sb("oneb", [128, 1], F32)
        S.op("dve", lambda e: e.memset(self.oneb[:], 1.0), writes=["oneb"])
        self.psT = self.pst("psT", [128, 1024], F32)
        self.ps = [self.pst(f"ps{i}", [128, 512], F32) for i in range(6)]

        S.dma("sp", "d_const", self.ident[:], self.ident_d, writes=["ident"])

        for s in range(self.n_seq):
            xin = self.x_in[s].rearrange("(tt p) d -> p tt d", p=128)
            for tt in range(NT):
                S.dma("sp", f"d_x{tt % 8}", self.x[:, tt, :], xin[:, tt, :], writes=[("x", tt)])
            for ph in self.phases:
                if ph == "attn":
                    self.attn_phase()
                elif ph == "ssm":
                    self.ssm_phase()
                elif ph == "moe0":
                    self.moe_phase(0)
                elif ph == "moe1":
                    self.moe_phase(1)
                elif ph == "final":
                    self.final_phase(s)
            if "final" not in self.phases:
                xo = self.out[s].rearrange("(tt p) d -> p tt d", p=128)
                for tt in range(NT):
                    S.dma("sp", f"d_o{tt % 4}", xo[:, tt, :], self.x[:, tt, :], reads=[("x", tt)])
        S.wait_keys("sp", [("x", tt) for tt in range(NT)] + ["fin0", "fin1"])
        return nc

    def norm_tile(self, tt, want32=None):
        S = self.S
        p = tt % 2
        sm = self.sm[p]
        hn = self.hn[p]
        xk = ("x", tt)
        S.op("act", lambda e: e.activation(out=hn[:], in_=self.x[:, tt, :], func=AF.Square, accum_out=sm[:, 0:1]),
             reads=[xk], writes=[("sm", p), ("hn", p)])
        S.op("act", lambda e: e.activation(out=sm[:, 1:2], in_=sm[:, 0:1], func=AF.Sqrt, scale=1.0 / D, bias=self.epsb[:, 0:1]),
             reads=[("sm", p), "epsb"], writes=[("sm", p)])
        S.op("dve", lambda e: e.reciprocal(out=sm[:, 2:3], in_=sm[:, 1:2]), reads=[("sm", p)], writes=[("sm", p)])
        S.op("dve", lambda e: e.scalar_tensor_tensor(out=hn[:], in0=self.x[:, tt, :], scalar=sm[:, 2:3], in1=self.lnw[:],
                                                     op0=ALU.mult, op1=ALU.mult),
             reads=[xk, ("sm", p), "lnw"], writes=[("hn", p)])
        if p == 0:
            banks = [(self.psT[:, 0:512], "psT"), (self.psT[:, 512:1024], "psT2")]
        else:
            banks = [(self.ps[0][:], ("ps", 0)), (self.ps[1][:], ("ps", 1))]
        for h, (bk, bkey) in enumerate(banks):
            for i in range(4):
                kc = 4 * h + i
                S.op("pe", lambda e, kc=kc, i=i, bk=bk: e.transpose(bk[:, i * 128:(i + 1) * 128], hn[:, kc * 128:(kc + 1) * 128], self.ident[:]),
                     reads=[("hn", p), "ident"], writes=[bkey], inc=(i == 3))
            S.op("act", lambda e, h=h, bk=bk: e.activation(out=self.hT[:, 4 * h:4 * h + 4, tt * 128:(tt + 1) * 128],
                                                           in_=bk.rearrange("p (k t) -> p k t", k=4), func=AF.Copy),
                 reads=[bkey], writes=[("hT", tt)])
            if want32 is not None:
                S.op("act", lambda e, h=h, bk=bk: e.activation(out=want32[0][:, 4 * h:4 * h + 4, :], in_=bk.rearrange("p (k t) -> p k t", k=4), func=AF.Copy),
                     reads=[bkey], writes=[want32[1]])

    def load_w(self, dst, src2d, key, KC, F):
        S = self.S
        g = max(1, 2048 // F)
        for k0 in range(0, KC, g):
            kn = min(g, KC - k0)
            si = self.stg_i % len(self.stg)
            self.stg_i += 1
            view = self.stg[si][:, 0:kn * F].rearrange("p (a b) -> p a b", a=kn)
            src = src2d[k0 * 128:(k0 + kn) * 128, :].rearrange("(kc p) f -> p kc f", p=128)
            S.dma("sp", f"d_stg{si}", view, src, writes=[("stg", si)])
            d = dst[:, k0:k0 + kn, :]
            if self.stg_i % 2 == 0:
                S.op("act", lambda en, d=d, view=view: en.activation(out=d, in_=view, func=AF.Copy), reads=[("stg", si)], writes=[key])
            else:
                S.op("pool", lambda en, d=d, view=view: en.tensor_copy(out=d, in_=view), reads=[("stg", si)], writes=[key])

    def attn_phase(self):
        S = self.S
        m0 = self.scope_begin()
        self.load_lnw(self.ln_mix[0:1, :])
        self.norm_all(self.norm_tile)
        self.scope_end(m0)
        KT = self.sb("KT", [128, 4, L], BF16)
        Va = self.sb("Vaug", [128, NT, 4, 65], BF16)
        Wq = self.sb("Wq", [128, 8, 1024], BF16)
        Wo = self.sb("Wo", [128, 8, 1024], BF16)
        identb = self.sb("identb", [128, 128], BF16)
        S.op("dve", lambda e: e.tensor_copy(out=identb[:], in_=self.ident[:]), reads=["ident"], writes=["identb"])
        S.op("dve", lambda e: e.memset(Va[:], 1.0), writes=["Va"])
        m1 = self.scope_begin()
        self.stg = [self.sb(f"stg{i}", [128, 2048], F32) for i in range(4)]
        self.stg_i = 0
        Wkv = self.sb("Wkv", [128, 8, 512], BF16)
        self.load_w(Wkv, self.w_qkv[:, 1024:1536], "Wkv", 8, 512)
        self.load_w(Wq, self.w_qkv[:, 0:1024], "Wq", 8, 1024)
        self.load_w(Wo, self.w_o, "Wo", 8, 1024)
        for ct in range(4):
            hk = [("hT", 4 * ct + i) for i in range(4)]
            for j in range(4):
                pk = self.ps[j % 2]
                self.mm_group(pk[0:64, :], [(Wkv[:, kc, j * 64:(j + 1) * 64], self.hT[:, kc, ct * 512:(ct + 1) * 512]) for kc in range(8)],
                              reads=hk + ["Wkv"], writes=[("ps", j % 2)])
                S.op("act", lambda e, pk=pk, j=j, ct=ct: e.activation(out=KT[0:64, j, ct * 512:(ct + 1) * 512], in_=pk[0:64, :], func=AF.Copy),
                     reads=[("ps", j % 2)], writes=["KT"])
        for tt in range(NT):
            pv = self.ps[2 + tt % 2]
            self.mm_group(pv[:, 0:256], [(self.hT[:, kc, tt * 128:(tt + 1) * 128], Wkv[:, kc, 256:512]) for kc in range(8)],
                          reads=[("hT", tt), "Wkv"], writes=[("ps", 2 + tt % 2)])
            S.op("act", lambda e, pv=pv, tt=tt: e.activation(out=Va[:, tt, :, 0:64], in_=pv[:, 0:256].rearrange("p (j d) -> p j d", j=4), func=AF.Copy),
                 reads=[("ps", 2 + tt % 2)], writes=["Va"])
        self.scope_end(m1)
        Tc = self.sb("Tc", [128, 16, 128], F32)
        Tp = self.sb("Tp", [128, 16, 128], F32)
        S.dma("sp", "d_const", Tc[:], self.tcur_d, writes=["Tc"])
        S.dma("sp", "d_const", Tp[:], self.tprev_d, writes=["Tp"])
        esk = self.sb("esk", [128, 16], F32)
        S.dma("sp", "d_const", esk[:], self.sinks[0:1, :].partition_broadcast(128), writes=["esk"])
        S.op("act", lambda e: e.activation(out=esk[:], in_=esk[:], func=AF.Exp), reads=["esk"], writes=["esk"])
        QT = [self.sb(f"QT{i}", [128, 16, 128], BF16) for i in range(2)]
        tA = [self.sb(f"tA{i}", [128, 512], F32) for i in range(2)]
        PA = [self.sb(f"PA{i}", [128, 512], BF16) for i in range(2)]
        PB = [self.sb(f"PB{i}", [128, 512], BF16) for i in range(2)]
        at = [self.sb(f"attn_tok{i}", [128, 16, 64], BF16) for i in range(2)]
        aT = self.sb("attnT", [128, 8, 128], BF16)
        den = self.sb("den", [128, 8], F32)
        ps = self.ps

        def st_q(b):
            qt, qk = QT[b % 2], ("QT", b % 2)
            pq = ps[4]
            for j in range(4):
                for g in range(4):
                    h = 4 * j + g
                    self.mm_group(pq[0:64, g * 128:(g + 1) * 128],
                                  [(Wq[:, kc, h * 64:(h + 1) * 64], self.hT[:, kc, b * 128:(b + 1) * 128]) for kc in range(8)],
                                  reads=[("hT", b), "Wq"], writes=[("ps", 4)])
                S.op("act", lambda e, j=j: e.activation(out=qt[0:64, 4 * j:4 * j + 4, :], in_=pq[0:64, :].rearrange("p (g t) -> p g t", g=4),
                                                        func=AF.Copy, scale=0.125),
                     reads=[("ps", 4)], writes=[qk])

        def st_s(b):
            qt, qk = QT[b % 2], ("QT", b % 2)
            ab, abk = at[b % 2], ("at", b % 2)
            for j in range(4):
                po = ps[2 + j % 2]
                pok = ("ps", 2 + j % 2)
                srcs = [(b, Tc, PA[j % 2], ("PA", j % 2), 0)]
                if b > 0:
                    srcs.append((b - 1, Tp, PB[j % 2], ("PB", j % 2), 1))
                for kb, T, P, pkey, w in srcs:
                    pa = ps[w]
                    S.op("pe", lambda e, pa=pa, kb=kb, j=j: e.matmul(pa[:], KT[0:64, j, kb * 128:(kb + 1) * 128], qt[0:64, 4 * j:4 * j + 4, :], start=True, stop=True),
                         reads=["KT", qk], writes=[("ps", w)])
                    S.op("dve", lambda e, pa=pa, T=T, w=w, j=j: e.tensor_tensor(out=tA[w][:], in0=pa[:], in1=T[:, 4 * j:4 * j + 4, :].rearrange("p g t -> p (g t)"), op=ALU.add),
                         reads=[("ps", w), "Tc", "Tp"], writes=[("tA", w)])
                    S.op("act", lambda e, P=P, w=w: e.activation(out=P[:], in_=tA[w][:], func=AF.Exp), reads=[("tA", w)], writes=[pkey])
                for g in range(4):
                    o = po[:, g * 65:(g + 1) * 65]
                    S.op("pe", lambda e, o=o, g=g, j=j: e.matmul(o, PA[j % 2][:, g * 128:(g + 1) * 128], Va[:, b, j, :], start=True, stop=(b == 0)),
                         reads=[("PA", j % 2), "Va"], writes=[pok], inc=(b == 0 and g == 3))
                    if b > 0:
                        S.op("pe", lambda e, o=o, g=g, j=j: e.matmul(o, PB[j % 2][:, g * 128:(g + 1) * 128], Va[:, b - 1, j, :], start=False, stop=True),
                             reads=[("PB", j % 2), "Va"], writes=[pok], inc=(g == 3))
                pov = po[:, 0:260].rearrange("p (g c) -> p g c", g=4)
                dj = den[:, 0:4]
                S.op("dve", lambda e, pov=pov, j=j: e.tensor_tensor(out=dj, in0=pov[:, :, 64], in1=esk[:, 4 * j:4 * j + 4], op=ALU.add),
                     reads=[pok, "esk"], writes=["den"])
                S.op("dve", lambda e: e.reciprocal(out=den[:, 4:8], in_=dj), reads=["den"], writes=["den"])
                S.op("dve", lambda e, pov=pov, j=j: e.tensor_tensor(out=ab[:, 4 * j:4 * j + 4, :], in0=pov[:, :, 0:64],
                                                                   in1=den[:, 4:8].unsqueeze(2).broadcast_to([128, 4, 64]), op=ALU.mult),
                     reads=[pok, "den"], writes=[abk])

        def st_o(b):
            ab, abk = at[b % 2], ("at", b % 2)
            for kc in range(8):
                S.op("pe", lambda e, kc=kc: e.matmul(self.psT[:, kc * 128:(kc + 1) * 128], ab[:].rearrange("p h d -> p (h d)")[:, kc * 128:(kc + 1) * 128],
                                                    identb[:], start=True, stop=True),
                     reads=[abk, "identb"], writes=["psT"], inc=(kc == 7))
            S.op("act", lambda e: e.activation(out=aT[:], in_=self.psT[:].rearrange("p (k t) -> p k t", k=8), func=AF.Copy), reads=["psT"], writes=["aT"])
            for half in range(2):
                pw = ps[5]
                self.mm_group(pw[:], [(aT[:, kc, :], Wo[:, kc, half * 512:(half + 1) * 512]) for kc in range(8)], reads=["aT", "Wo"], writes=[("ps", 5)])
                xs = self.x[:, b, half * 512:(half + 1) * 512]
                S.op("dve", lambda e, xs=xs: e.tensor_tensor(out=xs, in0=pw[:], in1=xs, op=ALU.add), reads=[("ps", 5), ("x", b)], writes=[("x", b)])

        for step in range(NT + 2):
            lists = []
            if 0 <= step - 2 < NT:
                lists.append(S.record(lambda bb=step - 2: st_o(bb)))
            if 0 <= step - 1 < NT:
                lists.append(S.record(lambda bb=step - 1: st_s(bb)))
            if step < NT:
                lists.append(S.record(lambda bb=step: st_q(bb)))
            S.emit_interleaved(lists)
        self.scope_end(m0)

    def ssm_phase(self):
        S = self.S
        m0 = self.scope_begin()
        self.load_lnw(self.ln_mix[1:2, :])
        self.norm_all(self.norm_tile)
        self.scope_end(m0)
        ps = self.ps
        sbf = self.sb
        identb = sbf("identb", [128, 128], BF16)
        S.op("dve", lambda e: e.tensor_copy(out=identb[:], in_=self.ident[:]), reads=["ident"], writes=["identb"])
        uinc = sbf("uinc", [128, 128], F32)
        ones = sbf("ones", [128, 128], F32)
        negm = sbf("negm", [128, 512], F32)
        cwT = sbf("cwT", [128, 24, 4], F32)
        cb = sbf("cb", [128, 24], F32)
        hcol = sbf("hcol", [32, 4], F32)
        dbc = sbf("dbc", [128, 32], F32)
        for t, src, k in ((uinc, self.uinc_d, "uinc"), (ones, self.ones_d, "ones"), (negm, self.negm_d, "negm"), (cwT, self.cwT_d, "cwT"),
                          (cb, self.cb_d, "cb"), (dbc, self.dsk_d[0:1, :].partition_broadcast(128), "dbc")):
            S.dma("sp", "d_const", t[:], src, writes=[k])
        S.dma("sp", "d_const", hcol[:, 0:2], self.hcol_d, writes=["hcol"])
        S.op("act", lambda e: e.activation(out=hcol[:, 2:3], in_=hcol[:, 1:2], func=AF.Exp), reads=["hcol"], writes=["hcol"])
        S.op("dve", lambda e: e.tensor_scalar(out=hcol[:, 3:4], in0=hcol[:, 2:3], scalar1=-1.0, scalar2=None, op0=ALU.mult), reads=["hcol"], writes=["hcol"])
        dt_tok = sbf("dt_tok", [128, NT, 32], F32)
        nacs = sbf("nacs", [128, NT, 32], F32)
        ea = sbf("ea", [128, NT, 32], F32)
        dec = sbf("dec", [128, NT, 32], F32)
        dtdte = sbf("dtdte", [128, NT, 32], F32)
        m1 = self.scope_begin()
        self.stg = [sbf(f"stg{i}", [128, 2048], F32) for i in range(2)]
        self.stg_i = 0
        Wdt = sbf("Wdt", [128, 8, 32], BF16)
        self.load_w(Wdt, self.w_in[:, 5120:5152], "Wdt", 8, 32)
        dtT = sbf("dtT", [128, L], F32)
        dAT = sbf("dAT", [128, L], F32)
        dA_tok = sbf("dA_tok", [128, NT, 32], F32)
        tmp = sbf("tmp_s", [128, NT, 32], F32)
        for ct in range(4):
            p = ps[ct % 2]
            sl = slice(ct * 512, (ct + 1) * 512)
            self.mm_group(p[0:32, :], [(Wdt[:, kc, :], self.hT[:, kc, sl]) for kc in range(8)],
                          reads=[("hT", 4 * ct + i) for i in range(4)] + ["Wdt"], writes=[("ps", ct % 2)])
            S.op("act", lambda e, p=p, sl=sl: e.activation(out=dtT[0:32, sl], in_=p[0:32, :], func=AF.Exp, bias=hcol[:, 0:1], scale=1.0),
                 reads=[("ps", ct % 2), "hcol"], writes=["dtT"])
            S.op("act", lambda e, sl=sl: e.activation(out=dtT[0:32, sl], in_=dtT[0:32, sl], func=AF.Ln, bias=self.oneb[0:32, 0:1], scale=1.0),
                 reads=["dtT", "oneb"], writes=["dtT"])
            S.op("dve", lambda e, sl=sl: e.tensor_scalar(out=dAT[0:32, sl], in0=dtT[0:32, sl], scalar1=hcol[:, 3:4], scalar2=None, op0=ALU.mult),
                 reads=["dtT", "hcol"], writes=["dAT"])
        for srcT, dst, k, bank in ((dtT, dt_tok, "dt_tok", 2), (dAT, dA_tok, "dA_tok", 3)):
            for c in range(NT):
                S.op("pe", lambda e, srcT=srcT, c=c, bank=bank: e.matmul(ps[bank][:, c * 32:(c + 1) * 32], srcT[0:32, c * 128:(c + 1) * 128],
                                                                        self.ident[0:32, 0:32], start=True, stop=True),
                     reads=["dtT", "dAT", "ident"], writes=[("ps", bank)], inc=(c == NT - 1))
            S.op("act", lambda e, dst=dst, bank=bank: e.activation(out=dst[:].rearrange("p c h -> p (c h)"), in_=ps[bank][:], func=AF.Copy),
                 reads=[("ps", bank)], writes=[k])
        for c in range(NT):
            S.op("pe", lambda e, c=c: e.matmul(ps[4][:, c * 32:(c + 1) * 32], uinc[:], dA_tok[:, c, :], start=True, stop=True),
                 reads=["uinc", "dA_tok"], writes=[("ps", 4)], inc=(c == NT - 1))
        for c in range(NT):
            S.op("pe", lambda e, c=c: e.matmul(ps[5][:, c * 32:(c + 1) * 32], ones[:], dA_tok[:, c, :], start=True, stop=True),
                 reads=["ones", "dA_tok"], writes=[("ps", 5)], inc=(c == NT - 1))
        fl = lambda t: t[:].rearrange("p c h -> p (c h)")
        S.op("act", lambda e: e.activation(out=fl(ea), in_=ps[4][:], func=AF.Exp), reads=[("ps", 4)], writes=["ea"])
        S.op("dve", lambda e: e.tensor_scalar(out=fl(nacs), in0=ps[4][:], scalar1=-1.0, scalar2=None, op0=ALU.mult), reads=[("ps", 4)], writes=["nacs"])
        S.op("act", lambda e: e.activation(out=fl(dec), in_=ps[5][:], func=AF.Exp), reads=[("ps", 5)], writes=["dec"])
        S.op("dve", lambda e: e.tensor_tensor(out=fl(tmp), in0=ps[5][:], in1=fl(nacs), op=ALU.add), reads=[("ps", 5), "nacs"], writes=["tmp_s"])
        S.op("act", lambda e: e.activation(out=fl(tmp), in_=fl(tmp), func=AF.Exp), reads=["tmp_s"], writes=["tmp_s"])
        S.op("dve", lambda e: e.tensor_tensor(out=fl(dtdte), in0=fl(tmp), in1=fl(dt_tok), op=ALU.mult), reads=["tmp_s", "dt_tok"], writes=["dtdte"])
        self.scope_end(m1)
        dA_tok2 = sbf("dA_tok2", [128, NT, 32], F32)
        abc = sbf("abc", [128, 32], F32)
        dg = sbf("dgA", [32, 32], F32)
        S.op("dve", lambda e: e.tensor_scalar(out=dg[:], in0=self.ident[0:32, 0:32], scalar1=hcol[:, 3:4], scalar2=None, op0=ALU.mult),
             reads=["ident", "hcol"], writes=["dgA"])
        S.op("pe", lambda e: e.matmul(ps[0][:, 0:32], ones[0:32, :], dg[:], start=True, stop=True), reads=["ones", "dgA"], writes=[("ps", 0)])
        S.op("act", lambda e: e.activation(out=abc[:], in_=ps[0][:, 0:32], func=AF.Copy), reads=[("ps", 0)], writes=["abc"])
        S.op("dve", lambda e: e.tensor_tensor(out=dA_tok2[:], in0=dt_tok[:], in1=abc[:].unsqueeze(1).broadcast_to([128, NT, 32]), op=ALU.mult),
             reads=["dt_tok", "abc"], writes=["dA_tok2"])
        dA_tok = dA_tok2

        for g in range(4):
            mg = self.scope_begin()
            Wz = sbf("Wz", [128, 8, 512], BF16)
            Wx = sbf("Wx", [128, 8, 512], BF16)
            WB = sbf("WB", [128, 8, 128], BF16)
            WC = sbf("WC", [128, 8, 128], BF16)
            Wout = sbf("Wout", [128, 4, 1024], BF16)
            nwb = sbf("nwb", [128, 512], F32)
            S.dma("sp", "d_const", nwb[:], self.nw_d[0:1, g * 512:(g + 1) * 512].partition_broadcast(128), writes=["nwb"])
            ms = self.scope_begin()
            self.stg = [sbf(f"stg{i}", [128, 2048], F32) for i in range(4)]
            self.stg_i = 0
            self.load_w(Wz, self.w_in[:, g * 512:(g + 1) * 512], "Wz", 8, 512)
            self.load_w(Wx, self.w_in[:, 2048 + g * 512:2048 + (g + 1) * 512], "Wx", 8, 512)
            self.load_w(WB, self.w_in[:, 4096 + g * 128:4096 + (g + 1) * 128], "WB", 8, 128)
            self.load_w(WC, self.w_in[:, 4608 + g * 128:4608 + (g + 1) * 128], "WC", 8, 128)
            self.load_w(Wout, self.w_out[g * 512:(g + 1) * 512, :], "Wout", 4, 1024)
            self.scope_end(ms)
            self.ssm_group(g, dict(Wz=Wz, Wx=Wx, WB=WB, WC=WC, Wout=Wout, nwb=nwb, identb=identb, uinc=uinc, ones=ones, negm=negm, cwT=cwT, cb=cb,
                                   dbc=dbc, dt_tok=dt_tok, nacs=nacs, ea=ea, dec=dec, dtdte=dtdte, dA_tok=dA_tok))
            self.scope_end(mg)
        self.scope_end(m0)

    def ssm_group(self, g, T):
        S = self.S
        ps = self.ps
        sbf = self.sb
        Wz, Wx, WB, WC, Wout, nwb, identb, uinc, ones, negm = (T[k] for k in ("Wz", "Wx", "WB", "WC", "Wout", "nwb", "identb", "uinc", "ones", "negm"))
        cwT, cb, dbc, dt_tok, nacs, ea, dec, dtdte, dA_tok = (T[k] for k in ("cwT", "cb", "dbc", "dt_tok", "nacs", "ea", "dec", "dtdte", "dA_tok"))
        xsT = sbf("xsT", [128, 4, 512], BF16)
        BT = sbf("BT", [128, 512], BF16)
        CT = [sbf(f"CT{i}", [128, 512], BF16) for i in range(2)]
        ub = [sbf(f"ub{i}", [128, 515], F32) for i in range(2)]
        acc = [sbf(f"acc{i}", [128, 512], F32) for i in range(2)]
        halo = sbf("halo", [128, 6, 3], F32)
        S.op("dve", lambda e: e.memset(halo[:], 0.0), writes=["halo"])
        dAU = sbf("dAU", [128, 8, 128], F32)
        DT = sbf("DT", [128, 8, 128], F32)
        MT = sbf("MT", [128, 8, 128], BF16)
        CBm = sbf("CBm", [128, 128], F32)
        nmb = sbf("nmb", [128, 8, 128], F32)
        Xt = sbf("Xt", [128, 8, 64], BF16)
        xsD = sbf("xsD", [128, 8, 64], BF16)
        Xd = [sbf(f"Xd{i}", [128, 8, 64], BF16) for i in range(2)]
        Btok = [sbf(f"Btok{i}", [128, 128], BF16) for i in range(2)]
        sz = [sbf(f"sz{i}", [128, 512], F32) for i in range(2)]
        S32 = sbf("S32", [128, 8, 64], F32)
        Sbf = sbf("Sbf", [128, 8, 64], BF16)
        Y = [sbf(f"Y{i}", [128, 8, 64], F32) for i in range(2)]
        sq = sbf("sq", [128, 512], F32)
        gn = sbf("gn", [128, 512], BF16)
        gnT = sbf("gnT", [128, 4, 128], BF16)
        S.op("dve", lambda e: e.memset(S32[:], 0.0), writes=["S32"])
        pyo = self.psT[:, 0:512]
        pcs = self.psT[:, 0:512]

        def phase_a(ct):
            sl = slice(ct * 512, (ct + 1) * 512)
            hk = [("hT", 4 * ct + i) for i in range(4)]
            ctb = CT[ct % 2]
            chunks_in = [(4 * g + i, ("xsT", i)) for i in range(4)] + [(16 + g, "BT"), (20 + g, ("CT", ct % 2))]
            for ci, (cidx, okey) in enumerate(chunks_in):
                q = ci % 2
                pp = ps[2 + q]
                if ci < 4:
                    W, wk, col = Wx, "Wx", slice(ci * 128, (ci + 1) * 128)
                elif ci == 4:
                    W, wk, col = WB, "WB", slice(0, 128)
                else:
                    W, wk, col = WC, "WC", slice(0, 128)
                self.mm_group(pp[:], [(W[:, kc, col], self.hT[:, kc, sl]) for kc in range(8)], reads=hk + [wk], writes=[("ps", 2 + q)])
                u, uk, a, ak = ub[q], ("ub", q), acc[q], ("acc", q)
                S.op("act", lambda e, u=u, ci=ci: e.activation(out=u[:, 0:3], in_=halo[:, ci, :], func=AF.Copy), reads=["halo"], writes=[uk])
                S.op("act", lambda e, u=u, pp=pp: e.activation(out=u[:, 3:515], in_=pp[:], func=AF.Copy), reads=[("ps", 2 + q)], writes=[uk])
                S.op("act", lambda e, u=u, ci=ci: e.activation(out=halo[:, ci, :], in_=u[:, 512:515], func=AF.Copy), reads=[uk], writes=["halo"])
                S.op("dve", lambda e, a=a, u=u, cidx=cidx: e.tensor_scalar(out=a[:], in0=u[:, 3:515], scalar1=cwT[:, cidx, 3:4], scalar2=cb[:, cidx:cidx + 1],
                                                                         op0=ALU.mult, op1=ALU.add), reads=[uk, "cwT", "cb"], writes=[ak])
                for w in (2, 1, 0):
                    S.op("dve", lambda e, a=a, u=u, cidx=cidx, w=w: e.scalar_tensor_tensor(out=a[:], in0=u[:, w:w + 512], scalar=cwT[:, cidx, w:w + 1], in1=a[:],
                                                                                      op0=ALU.mult, op1=ALU.add), reads=[uk, "cwT", ak], writes=[ak])
                dst = xsT[:, ci, :] if ci < 4 else (BT[:] if ci == 4 else ctb[:])
                S.op("act", lambda e, a=a, dst=dst: e.activation(out=dst, in_=a[:], func=AF.Silu), reads=[ak], writes=[okey])

        def stage1(c):
            ct, cq = divmod(c, 4)
            cs = slice(cq * 128, (cq + 1) * 128)
            hs = slice(8 * g, 8 * g + 8)
            par = c % 2
            ctb, ctk = CT[ct % 2], ("CT", ct % 2)
            xk = [("xsT", i) for i in range(4)]
            yb, ybk = ps[par], ("ps", par)
            bc = lambda t: t[:, c, hs].unsqueeze(2).broadcast_to([128, 8, 64])
            abank = [(ps[4][:], ("ps", 4)), (self.psT[:, 512:1024], "psT2")]
            S.op("dve", lambda e: e.tensor_tensor(out=dAU[:], in0=uinc[:].unsqueeze(1).broadcast_to([128, 8, 128]),
                                                  in1=dA_tok[:, c, hs].unsqueeze(2).broadcast_to([128, 8, 128]), op=ALU.mult),
                 reads=["uinc", "dA_tok2"], writes=["dAU"])
            S.op("dve", lambda e: e.tensor_tensor(out=nmb[:], in0=negm[:].rearrange("p (h l) -> p h l", h=4)[:, 0:1, :].broadcast_to([128, 8, 128]),
                                                  in1=nacs[:, c, hs].unsqueeze(2).broadcast_to([128, 8, 128]), op=ALU.add),
                 reads=["negm", "nacs"], writes=["nmb"])
            self.mm_group(ps[3][:], [(self.hT[:, kc, c * 128:(c + 1) * 128], Wz[:, kc, :]) for kc in range(8)],
                          reads=[("hT", c), "Wz"], writes=[("ps", 3)])
            S.op("act", lambda e: e.activation(out=sz[par][:], in_=ps[3][:], func=AF.Silu), reads=[("ps", 3)], writes=[("sz", par)])
            for hh in range(2):
                pb, pbk = abank[hh]
                S.op("pe", lambda e, hh=hh, pb=pb: e.matmul(pb, ones[:], dAU[:, 4 * hh:4 * hh + 4, :], start=True, stop=False),
                     reads=["ones", "dAU"], writes=[pbk], inc=False)
                S.op("pe", lambda e, hh=hh, pb=pb: e.matmul(pb, self.ident[:], nmb[:, 4 * hh:4 * hh + 4, :], start=False, stop=True),
                     reads=["ident", "nmb"], writes=[pbk])
                S.op("act", lambda e, hh=hh, pb=pb: e.activation(out=DT[:, 4 * hh:4 * hh + 4, :].rearrange("p h l -> p (h l)"), in_=pb, func=AF.Exp),
                     reads=[pbk], writes=[("DT", hh)])
            for i in range(4):
                S.op("pe", lambda e, i=i: e.matmul(ps[2][:, i * 128:(i + 1) * 128], xsT[:, i, cs], identb[:], start=True, stop=True),
                     reads=xk + ["identb"], writes=[("ps", 2)], inc=(i == 3))
            S.op("pe", lambda e: e.matmul(ps[3][:, 0:128], BT[:, cs], identb[:], start=True, stop=True), reads=["BT", "identb"], writes=[("ps", 3)], inc=False)
            S.op("pe", lambda e: e.matmul(ps[3][:, 128:256], BT[:, cs], ctb[:, cs], start=True, stop=True), reads=["BT", ctk], writes=[("ps", 3)])
            xv = ps[2][:].rearrange("p (h d) -> p h d", h=8)
            S.op("dve", lambda e: e.tensor_tensor(out=CBm[:], in0=ps[3][:, 128:256], in1=uinc[:], op=ALU.mult), reads=[("ps", 3), "uinc"], writes=["CBm"])
            S.op("act", lambda e: e.activation(out=Btok[par][:], in_=ps[3][:, 0:128], func=AF.Copy), reads=[("ps", 3)], writes=[("Btok", par)])
            for hh in range(2):
                S.op("dve", lambda e, hh=hh: e.tensor_tensor(out=MT[:, 4 * hh:4 * hh + 4, :], in0=DT[:, 4 * hh:4 * hh + 4, :],
                                                             in1=CBm[:].unsqueeze(1).broadcast_to([128, 4, 128]), op=ALU.mult),
                     reads=[("DT", hh), "CBm"], writes=[("MT", hh)])
            S.op("dve", lambda e: e.tensor_tensor(out=Xt[:], in0=xv, in1=bc(dt_tok), op=ALU.mult), reads=[("ps", 2), "dt_tok"], writes=["Xt"])
            S.op("dve", lambda e: e.tensor_tensor(out=xsD[:], in0=xv, in1=dbc[:, hs].unsqueeze(2).broadcast_to([128, 8, 64]), op=ALU.mult),
                 reads=[("ps", 2), "dbc"], writes=["xsD"])
            S.op("dve", lambda e: e.tensor_tensor(out=Xd[par][:], in0=xv, in1=bc(dtdte), op=ALU.mult), reads=[("ps", 2), "dtdte"], writes=[("Xd", par)])
            S.op("pe", lambda e: e.matmul(yb[:], identb[:], xsD[:].rearrange("p h d -> p (h d)"), start=True, stop=False),
                 reads=["identb", "xsD"], writes=[ybk], inc=False)
            for hl in range(8):
                S.op("pe", lambda e, hl=hl: e.matmul(yb[:, hl * 64:(hl + 1) * 64], MT[:, hl, :], Xt[:, hl, :], start=False, stop=(hl == 7)),
                     reads=[("MT", hl // 4), "Xt"], writes=[ybk], inc=(hl == 7))

        def stage2a(c):
            ct, cq = divmod(c, 4)
            cs = slice(cq * 128, (cq + 1) * 128)
            hs = slice(8 * g, 8 * g + 8)
            par = c % 2
            ctb, ctk = CT[ct % 2], ("CT", ct % 2)
            yb, ybk = ps[par], ("ps", par)
            Yc, Yk = Y[par], ("Y", par)
            bc = lambda t: t[:, c, hs].unsqueeze(2).broadcast_to([128, 8, 64])
            Yf = Yc[:].rearrange("p h d -> p (h d)")
            if c > 0:
                S.op("pe", lambda e: e.matmul(pyo, ctb[:, cs], Sbf[:].rearrange("p h d -> p (h d)"), start=True, stop=True),
                     reads=[ctk, "Sbf"], writes=["psT"])
                S.op("dve", lambda e: e.tensor_tensor(out=Yc[:], in0=pyo.rearrange("p (h d) -> p h d", h=8), in1=bc(ea), op=ALU.mult),
                     reads=["psT", "ea"], writes=[Yk])
            if c < NT - 1:
                S.op("pe", lambda e: e.matmul(pcs, Btok[par][:], Xd[par][:].rearrange("p h d -> p (h d)"), start=True, stop=True),
                     reads=[("Btok", par), ("Xd", par)], writes=["psT"])
                S.op("dve", lambda e: e.tensor_tensor(out=S32[:], in0=S32[:], in1=bc(dec), op=ALU.mult), reads=["S32", "dec"], writes=["S32"])
                S.op("dve", lambda e: e.tensor_tensor(out=S32[:].rearrange("p h d -> p (h d)"), in0=pcs, in1=S32[:].rearrange("p h d -> p (h d)"), op=ALU.add),
                     reads=["psT", "S32"], writes=["S32"])
                S.op("act", lambda e: e.activation(out=Sbf[:], in_=S32[:], func=AF.Copy), reads=["S32"], writes=["Sbf"])
            if c > 0:
                S.op("dve", lambda e: e.tensor_tensor(out=Yf, in0=yb[:], in1=Yf, op=ALU.add), reads=[ybk, Yk], writes=[Yk])
            else:
                S.op("act", lambda e: e.activation(out=Yf, in_=yb[:], func=AF.Copy), reads=[ybk], writes=[Yk])

        def stage2b(c):
            par = c % 2
            Yc, Yk = Y[par], ("Y", par)
            Yf = Yc[:].rearrange("p h d -> p (h d)")
            S.op("dve", lambda e: e.tensor_tensor(out=Yf, in0=Yf, in1=sz[par][:], op=ALU.mult), reads=[Yk, ("sz", par)], writes=[Yk])
            sm = self.sm[par]
            smk = ("sm", par)
            S.op("act", lambda e: e.activation(out=sq[:], in_=Yf, func=AF.Square, accum_out=sm[:, 0:1]), reads=[Yk], writes=["sq", smk])
            S.op("act", lambda e: e.activation(out=sm[:, 1:2], in_=sm[:, 0:1], func=AF.Sqrt, scale=1.0 / 512, bias=self.epsb[:, 0:1]), reads=[smk, "epsb"], writes=[smk])
            S.op("dve", lambda e: e.reciprocal(out=sm[:, 2:3], in_=sm[:, 1:2]), reads=[smk], writes=[smk])
            S.op("dve", lambda e: e.scalar_tensor_tensor(out=gn[:], in0=Yf, scalar=sm[:, 2:3], in1=nwb[:], op0=ALU.mult, op1=ALU.mult),
                 reads=[Yk, smk, "nwb"], writes=["gn"])
            for i in range(4):
                S.op("pe", lambda e, i=i: e.matmul(ps[5][:, i * 128:(i + 1) * 128], gn[:, i * 128:(i + 1) * 128], identb[:], start=True, stop=True),
                     reads=["gn", "identb"], writes=[("ps", 5)], inc=(i == 3))
            S.op("act", lambda e: e.activation(out=gnT[:], in_=ps[5][:].rearrange("p (k t) -> p k t", k=4), func=AF.Copy), reads=[("ps", 5)], writes=["gnT"])
            for half in range(2):
                self.mm_group(ps[5][:], [(gnT[:, i, :], Wout[:, i, half * 512:(half + 1) * 512]) for i in range(4)],
                              reads=["gnT", "Wout"], writes=[("ps", 5)])
                xs_ = self.x[:, c, half * 512:(half + 1) * 512]
                S.op("dve", lambda e, xs_=xs_: e.tensor_tensor(out=xs_, in0=ps[5][:], in1=xs_, op=ALU.add), reads=[("ps", 5), ("x", c)], writes=[("x", c)])

        phase_a(0)
        S.emit_interleaved([S.record(lambda: stage1(0))])
        for c in range(NT + 1):
            lists = []
            if c < NT:
                lists.append(S.record(lambda c=c: stage2a(c)))
            if c >= 1:
                lists.append(S.record(lambda c=c: stage2b(c - 1)))
            if c + 1 < NT:
                if (c + 1) % 4 == 0:
                    phase_a((c + 1) // 4)
                lists.append(S.record(lambda c=c: stage1(c + 1)))
            S.emit_interleaved(lists)

    def router_tile(self, li, tt):
        S = self.S
        p = tt % 2
        sm = self.sm[p]
        k = ("sm", p)
        h32 = self.h32[p]
        psR = self.ps[4 + p]
        self.mm_group(psR[:, 0:72], [(h32[:, kc, :], self.wr[:, kc, :]) for kc in range(8)],
                      reads=[("h32", p), "wr"], writes=[("ps", 4 + p)])
        lg = sm[:, 8:80]
        V = lambda fn, r=(), w=(): S.op("dve", fn, reads=[k] + list(r), writes=[k] + list(w))
        V(lambda e: e.tensor_tensor(out=lg, in0=psR[:, 0:72], in1=self.rb[:], op=ALU.add), r=[("ps", 4 + p), "rb"])
        gmax, ngmax, gsum, gp = sm[:, 80:81], sm[:, 81:82], sm[:, 82:83], sm[:, 83:84]
        V(lambda e: e.tensor_reduce(out=gmax, in_=lg[:, 0:8], axis=AX.X, op=ALU.max))
        V(lambda e: e.tensor_scalar(out=ngmax, in0=gmax, scalar1=-1.0, scalar2=None, op0=ALU.mult))
        S.op("act", lambda e: e.activation(out=sm[:, 96:104], in_=lg[:, 0:8], func=AF.Exp, bias=ngmax, scale=1.0, accum_out=gsum),
             reads=[k], writes=[k])
        V(lambda e: e.reciprocal(out=gp, in_=gsum))
        gmask = sm[:, 104:112]
        V(lambda e: e.tensor_scalar(out=gmask, in0=lg[:, 0:8], scalar1=gmax, scalar2=None, op0=ALU.is_equal))
        negb = sm[:, 112:120]
        V(lambda e: e.tensor_scalar(out=negb, in0=gmask, scalar1=1.0, scalar2=1e30, op0=ALU.subtract, op1=ALU.mult))
        elm = sm[:, 128:192]
        V(lambda e: e.tensor_tensor(out=elm.rearrange("p (g j) -> p g j", g=8), in0=lg[:, 8:72].rearrange("p (g j) -> p g j", g=8),
                                    in1=negb.unsqueeze(2).broadcast_to([128, 8, 8]), op=ALU.add))
        m1, m2, dd, ed, s1, g1, g2 = (sm[:, 84 + i:85 + i] for i in range(7))
        V(lambda e: e.tensor_reduce(out=m1, in_=elm, axis=AX.X, op=ALU.max))
        mask1 = sm[:, 192:256]
        V(lambda e: e.tensor_scalar(out=mask1, in0=elm, scalar1=m1, scalar2=None, op0=ALU.is_equal))
        elm2 = sm[:, 256:320]
        V(lambda e: e.scalar_tensor_tensor(out=elm2, in0=mask1, scalar=-1e30, in1=elm, op0=ALU.mult, op1=ALU.add))
        V(lambda e: e.tensor_reduce(out=m2, in_=elm2, axis=AX.X, op=ALU.max))
        mask2 = sm[:, 320:384]
        V(lambda e: e.tensor_scalar(out=mask2, in0=elm2, scalar1=m2, scalar2=None, op0=ALU.is_equal))
        V(lambda e: e.tensor_tensor(out=dd, in0=m2, in1=m1, op=ALU.subtract))
        S.op("act", lambda e: e.activation(out=ed, in_=dd, func=AF.Exp), reads=[k], writes=[k])
        V(lambda e: e.tensor_scalar(out=ed, in0=ed, scalar1=1.0, scalar2=None, op0=ALU.add))
        V(lambda e: e.reciprocal(out=s1, in_=ed))
        V(lambda e: e.tensor_tensor(out=g1, in0=gp, in1=s1, op=ALU.mult))
        V(lambda e: e.tensor_tensor(out=g2, in0=gp, in1=g1, op=ALU.subtract))
        V(lambda e: e.tensor_scalar(out=mask1, in0=mask1, scalar1=g1, scalar2=None, op0=ALU.mult))
        V(lambda e: e.scalar_tensor_tensor(out=self.G[:, tt, :], in0=mask2, scalar=g2, in1=mask1, op0=ALU.mult, op1=ALU.add),
          w=[("G", tt)])

    def router_batched(self, LG):
        S = self.S
        sbf = self.sb
        B4 = [128, NT, 8, 8]
        B3 = [128, NT, 8]
        gl = LG[:, :, 0:8]
        el = LG[:, :, 8:72].rearrange("p t (g j) -> p t g j", g=8)
        t16 = sbf("r_t16", [128, 12, NT], F32)
        gmax, gsum, gp, m1, m2, dd, ed, s1, g1, g2 = (t16[:, i, :] for i in range(10))
        gsh = sbf("r_gsh", B3, F32)
        gex = sbf("r_gex", B3, F32)
        gmask = sbf("r_gmask", B3, F32)
        elm = sbf("r_elm", [128, NT, 64], F32)
        mask1 = sbf("r_mask1", [128, NT, 64], F32)
        elm2 = sbf("r_elm2", [128, NT, 64], F32)
        mask2 = sbf("r_mask2", [128, NT, 64], F32)
        k = "rtr"
        V = lambda fn, r=(), w=(): S.op("dve", fn, reads=[k] + list(r), writes=[k] + list(w))
        A = lambda fn: S.op("act", fn, reads=[k], writes=[k])
        b3 = lambda v: v.unsqueeze(2).broadcast_to(B3)
        b64 = lambda v: v.unsqueeze(2).broadcast_to([128, NT, 64])
        f2 = lambda t: t[:].rearrange("p t e -> p (t e)")
        lgk = [("LG", tt) for tt in range(NT)]
        V(lambda e: e.tensor_reduce(out=gmax, in_=gl, axis=AX.X, op=ALU.max), r=lgk)
        V(lambda e: e.tensor_tensor(out=gsh[:], in0=gl, in1=b3(gmax), op=ALU.subtract), r=lgk)
        A(lambda e: e.activation(out=gex[:], in_=gsh[:], func=AF.Exp))
        V(lambda e: e.tensor_reduce(out=gsum, in_=gex[:], axis=AX.X, op=ALU.add))
        V(lambda e: e.reciprocal(out=gp, in_=gsum))
        V(lambda e: e.tensor_scalar(out=gmask[:], in0=gsh[:], scalar1=0.0, scalar2=None, op0=ALU.is_equal))
        V(lambda e: e.tensor_scalar(out=gmask[:], in0=gmask[:], scalar1=1.0, scalar2=1e30, op0=ALU.subtract, op1=ALU.mult))
        V(lambda e: e.tensor_tensor(out=elm[:].rearrange("p t (g j) -> p t g j", g=8), in0=el, in1=gmask[:].unsqueeze(3).broadcast_to(B4), op=ALU.add), r=lgk)
        V(lambda e: e.tensor_reduce(out=m1, in_=elm[:], axis=AX.X, op=ALU.max))
        V(lambda e: e.tensor_tensor(out=mask1[:], in0=elm[:], in1=b64(m1), op=ALU.is_equal))
        V(lambda e: e.scalar_tensor_tensor(out=f2(elm2), in0=f2(mask1), scalar=-1e30, in1=f2(elm), op0=ALU.mult, op1=ALU.add))
        V(lambda e: e.tensor_reduce(out=m2, in_=elm2[:], axis=AX.X, op=ALU.max))
        V(lambda e: e.tensor_tensor(out=mask2[:], in0=elm2[:], in1=b64(m2), op=ALU.is_equal))
        V(lambda e: e.tensor_tensor(out=dd, in0=m2, in1=m1, op=ALU.subtract))
        A(lambda e: e.activation(out=ed, in_=dd, func=AF.Exp))
        V(lambda e: e.tensor_scalar(out=ed, in0=ed, scalar1=1.0, scalar2=None, op0=ALU.add))
        V(lambda e: e.reciprocal(out=s1, in_=ed))
        V(lambda e: e.tensor_tensor(out=g1, in0=gp, in1=s1, op=ALU.mult))
        V(lambda e: e.tensor_tensor(out=g2, in0=gp, in1=g1, op=ALU.subtract))
        V(lambda e: e.tensor_tensor(out=mask1[:], in0=mask1[:], in1=b64(g1), op=ALU.mult))
        V(lambda e: e.tensor_tensor(out=mask2[:], in0=mask2[:], in1=b64(g2), op=ALU.mult))
        V(lambda e: e.tensor_tensor(out=self.G[:], in0=mask1[:], in1=mask2[:], op=ALU.add), w=[("G", tt) for tt in range(NT)])

    def load_expert(self, li, e, slot):
        S = self.S
        pieces = []
        for nm, w, dst in (("Wg", self.w_gate, self.Wg[slot]), ("Wu", self.w_up, self.Wu[slot])):
            for h in range(2):
                src = w[li, e, h * 512:(h + 1) * 512, :].rearrange("(kc p) f -> p kc f", p=128)
                pieces.append((src, dst[:, h * 4:(h + 1) * 4, :], (nm, slot), [128, 4, 512]))
        for h in range(2):
            src = self.w_down[li, e, h * 256:(h + 1) * 256, :].rearrange("(kc p) f -> p kc f", p=128)
            pieces.append((src, self.Wd[slot][:, h * 2:(h + 1) * 2, :], ("Wd", slot), [128, 2, 1024]))
        for src, dst, key, shp in pieces:
            si = self.stg_i % 2
            self.stg_i += 1
            stg = self.stg[si]
            view = stg[:].rearrange("p (a b) -> p a b", a=shp[1])
            S.dma("sp", f"d_stg{si}", view, src, writes=[("stg", si)])
            S.op("pool", lambda en, dst=dst, view=view: en.tensor_copy(out=dst, in_=view), reads=[("stg", si)], writes=[key])

    def moe_phase(self, li):
        S = self.S
        m0 = self.scope_begin()
        self.G = self.sb("G", [128, NT, NE], F32)
        m1 = self.scope_begin()
        self.load_lnw(self.ln_ffn[li:li + 1, :])
        self.wr = self.sb("wr", [128, 8, 72], F32)
        self.rb = self.sb("rb", [128, 72], F32)
        S.dma("sp", "d_const", self.wr[:], self.w_router[li].rearrange("(kc p) e -> p kc e", p=128), writes=["wr"])
        S.dma("sp", "d_const", self.rb[:], self.b_router[li:li + 1, :].partition_broadcast(128), writes=["rb"])
        self.h32 = [self.sb(f"h32_{i}", [128, 8, 128], F32) for i in range(2)]
        LG = self.sb("LG", [128, NT, 72], F32)
        def rt(tt):
            p = tt % 2
            self.norm_tile(tt, want32=(self.h32[p], ("h32", p)))
            psR = self.ps[4 + p]
            self.mm_group(psR[:, 0:72], [(self.h32[p][:, kc, :], self.wr[:, kc, :]) for kc in range(8)],
                          reads=[("h32", p), "wr"], writes=[("ps", 4 + p)])
            S.op("dve", lambda e: e.tensor_tensor(out=LG[:, tt, :], in0=psR[:, 0:72], in1=self.rb[:], op=ALU.add),
                 reads=[("ps", 4 + p), "rb"], writes=[("LG", tt)])
        self.norm_all(rt)
        self.router_batched(LG)
        self.scope_end(m1)
        self.Wg = [self.sb(f"Wg{i}", [128, 8, FF], BF16) for i in range(2)]
        self.Wu = [self.sb(f"Wu{i}", [128, 8, FF], BF16) for i in range(2)]
        self.Wd = [self.sb(f"Wd{i}", [128, 4, D], BF16) for i in range(2)]
        self.stg = [self.sb(f"stg{i}", [128, 2048], F32) for i in range(2)]
        self.sg = [self.sb(f"sg{i}", [128, 512], BF16) for i in range(2)]
        self.hid = [self.sb(f"hid{i}", [128, 4, 512], BF16) for i in range(2)]
        self.stg_i = 0
        nE = self.n_exp
        obanks = [(self.ps[4][:], ("ps", 4)), (self.ps[5][:], ("ps", 5)), (self.psT[:, 0:512], "psT"), (self.psT[:, 512:1024], "psT2")]

        def gate_up(e, ct):
            slot = e % 2
            Wg, Wu = self.Wg[slot], self.Wu[slot]
            hid = self.hid[ct % 2]
            hkeys = [("hT", 4 * ct + i) for i in range(4)]
            rhs = [self.hT[:, kc, ct * 512:(ct + 1) * 512] for kc in range(8)]
            for fc in range(4):
                q = fc % 2
                pg, pu = self.ps[q], self.ps[2 + q]
                self.mm_group(pg[:], [(Wg[:, kc, fc * 128:(fc + 1) * 128], rhs[kc]) for kc in range(8)], reads=hkeys + [("Wg", slot)], writes=[("ps", q)])
                self.mm_group(pu[:], [(Wu[:, kc, fc * 128:(fc + 1) * 128], rhs[kc]) for kc in range(8)], reads=hkeys + [("Wu", slot)], writes=[("ps", 2 + q)])
                S.op("act", lambda en, q=q, pg=pg: en.activation(out=self.sg[q][:], in_=pg[:], func=AF.Silu), reads=[("ps", q)], writes=[("sg", q)])
                S.op("dve", lambda en, q=q, pu=pu, fc=fc: en.tensor_tensor(out=hid[:, fc, :], in0=pu[:], in1=self.sg[q][:], op=ALU.mult),
                     reads=[("ps", 2 + q), ("sg", q)], writes=[("hid", ct % 2, fc)])

        def down(e, ct):
            slot = e % 2
            Wd = self.Wd[slot]
            hid = self.hid[ct % 2]
            for tq in range(4):
                tt = ct * 4 + tq
                for half in range(2):
                    po, pk = obanks[(tq * 2 + half) % 4]
                    self.mm_group(po, [(hid[:, fc, tq * 128:(tq + 1) * 128], Wd[:, fc, half * 512:(half + 1) * 512]) for fc in range(4)],
                                  reads=[("hid", ct % 2, fc) for fc in range(4)] + [("Wd", slot)], writes=[pk])
                    xs = self.x[:, tt, half * 512:(half + 1) * 512]
                    S.op("dve", lambda en, po=po, xs=xs, tt=tt: en.scalar_tensor_tensor(
                        out=xs, in0=po, scalar=self.G[:, tt, e:e + 1], in1=xs, op0=ALU.mult, op1=ALU.add),
                         reads=[pk, ("G", tt), ("xh", tt, half)], writes=[("xh", tt, half)])

        units = [(e, ct) for e in range(nE) for ct in range(4)]
        self.load_expert(li, 0, 0)
        if nE > 1:
            self.load_expert(li, 1, 1)
        gate_up(*units[0])
        for i, (e, ct) in enumerate(units):
            if i + 1 < len(units):
                ne, nct = units[i + 1]
                gate_up(ne, nct)
            down(e, ct)
            if ct == 3 and e + 2 < nE:
                self.load_expert(li, e + 2, e % 2)
        self.scope_end(m0)

    def final_phase(self, s):
        S = self.S
        xo = self.out[s].rearrange("(tt p) d -> p tt d", p=128)
        mf = self.scope_begin()
        self.load_lnw(self.ln_final[0:1, :])
        for tt in range(NT):
            p = tt % 2
            sm = self.sm[p]
            hn = self.hn[p]
            xk = ("x", tt)
            S.op("act", lambda e: e.activation(out=hn[:], in_=self.x[:, tt, :], func=AF.Square, accum_out=sm[:, 0:1]),
                 reads=[xk], writes=[("sm", p), ("hn", p)])
            S.op("act", lambda e: e.activation(out=sm[:, 1:2], in_=sm[:, 0:1], func=AF.Sqrt, scale=1.0 / D, bias=self.epsb[:, 0:1]),
                 reads=[("sm", p), "epsb"], writes=[("sm", p)])
            S.op("dve", lambda e: e.reciprocal(out=sm[:, 2:3], in_=sm[:, 1:2]), reads=[("sm", p)], writes=[("sm", p)])
            S.op("dve", lambda e: e.scalar_tensor_tensor(out=hn[:], in0=self.x[:, tt, :], scalar=sm[:, 2:3], in1=self.lnw[:],
                                                         op0=ALU.mult, op1=ALU.mult),
                 reads=[xk, ("sm", p), "lnw"], writes=[("hn", p)])
            S.dma("sp", f"d_o{p}", xo[:, tt, :], hn[:], reads=[("hn", p)], writes=[f"fin{p}"])
        self.scope_end(mf)


def _consts():
    return {"ident": np.eye(128, dtype=np.float32)}


def _bucket_table():
    n = np.arange(128)
    nf = np.maximum(n, 1).astype(np.float32)
    large = 16 + (np.log(nf / np.float32(16)) / np.float32(np.log(128 / 16)) * np.float32(16)).astype(np.int32)
    large = np.minimum(large, 31)
    return np.where(n < 16, n, large)


def _bias_tables(rel_bias):
    bt = _bucket_table()
    k = np.arange(128)[:, None]
    q = np.arange(128)[None, :]
    rb = np.asarray(rel_bias, dtype=np.float32)
    dc = q - k
    vc = dc >= 0
    tc = rb[bt[np.where(vc, dc, 0)]]
    tc = np.where(vc[:, :, None], tc, np.float32(NEG))
    dp = q + 128 - k
    vp = dp < 128
    tp = rb[bt[np.where(vp, dp, 0)]]
    tp = np.where(vp[:, :, None], tp, np.float32(NEG))
    return (np.ascontiguousarray(tc.transpose(0, 2, 1), dtype=np.float32),
            np.ascontiguousarray(tp.transpose(0, 2, 1), dtype=np.float32))


def make_in_maps(inputs, n_seq=2, n_cores=8, phases=("attn", "moe0", "ssm", "moe1", "final")):
    x = np.ascontiguousarray(inputs["x"], dtype=np.float32)
    w_router = np.ascontiguousarray(np.concatenate([inputs["moe_w_group"], inputs["moe_w_expert"]], axis=2), dtype=np.float32)
    b_router = np.ascontiguousarray(np.concatenate([inputs["moe_b_group"], inputs["moe_b_expert"]], axis=1), dtype=np.float32)
    shared = {
        "ln_mix": inputs["ln_mix"], "ln_ffn": inputs["ln_ffn"], "ln_final": np.asarray(inputs["ln_final"]).reshape(1, D),
        "w_router": w_router, "b_router": b_router,
        "moe_w_gate": inputs["moe_w_gate"], "moe_w_up": inputs["moe_w_up"], "moe_w_down": inputs["moe_w_down"],
    }
    if "attn_w_qkv" in inputs and "attn" in phases:
        tc, tp = _bias_tables(inputs["rel_bias"])
        shared.update({"attn_w_qkv": np.asarray(inputs["attn_w_qkv"]).reshape(D, 1536), "attn_w_o": np.asarray(inputs["attn_w_o"]).reshape(D, D),
                       "attn_sinks": np.asarray(inputs["attn_sinks"]).reshape(1, 16), "tcur": tc, "tprev": tp})
    if "ssm_w_in" in inputs and "ssm" in phases:
        cw = np.asarray(inputs["ssm_conv_w"], dtype=np.float32).reshape(4, 3072)
        k_ = np.arange(128)[:, None]
        l_ = np.arange(128)[None, :]
        shared.update({
            "ssm_w_in": np.asarray(inputs["ssm_w_in"]).reshape(D, 5152), "ssm_w_out": np.asarray(inputs["ssm_w_out"]).reshape(2048, D),
            "ssm_cwT": cw.T.reshape(24, 128, 4).transpose(1, 0, 2),
            "ssm_cb": np.asarray(inputs["ssm_conv_b"], dtype=np.float32).reshape(24, 128).T,
            "ssm_hcol": np.stack([np.asarray(inputs["ssm_dt_bias"]).reshape(32), np.asarray(inputs["ssm_a_log"]).reshape(32)], axis=1),
            "ssm_d": np.asarray(inputs["ssm_d"]).reshape(1, 32), "ssm_norm_w": np.asarray(inputs["ssm_norm_w"]).reshape(1, 2048),
            "c_uincl": (k_ <= l_).astype(np.float32), "c_ones": np.ones((128, 128), np.float32),
            "c_negmask4": np.tile(np.where(k_ > l_, np.float32(NEG), np.float32(0.0)), (1, 4)),
        })
    shared = {k: np.ascontiguousarray(v, dtype=np.float32) for k, v in shared.items()}
    shared.update(_consts())
    maps = []
    for c in range(n_cores):
        m = dict(shared)
        m["x"] = x[c * n_seq:(c + 1) * n_seq]
        maps.append(m)
    return maps


def kernel(**inputs):
    kb = K()
    nc = kb.build()
    in_maps = make_in_maps(inputs)
    res = run_bass_kernel_spmd(nc, in_maps, core_ids=list(range(8)))
    return np.concatenate([r["out"] for r in res.results], axis=0).astype(np.float32)
```
